# Optimizing a Trainium2 kernel written in Bass

```python
import math
import jax, jax.numpy as jnp
from jax import lax
import numpy as np

D_MODEL = 4096
BATCH = 2
SEQ = 8192
DEPTH = 1

ATT_HEADS = 16
HEAD_DIM = 128
ATT_WIDTH = ATT_HEADS * HEAD_DIM
DIL_PAIRS = ((128, 1), (512, 4), (2048, 16))
NUM_BUCKETS = 32
MAX_DISTANCE = 1024
POOL_WINDOWS = (2, 4, 8, 16)
POOL_WIDTH = D_MODEL - ATT_WIDTH
POOL_GROUP = POOL_WIDTH // len(POOL_WINDOWS)
MIX_WIDTH = ATT_WIDTH + POOL_WIDTH
IN_WIDTH = 3 * ATT_WIDTH + POOL_WIDTH
PEER_HEADS = 8
PEER_QDIM = 256
N_KEYS = 128
N_EXPERTS = N_KEYS * N_KEYS
PEER_TOPK = 16
PEER_CHUNK = 64
RMS_EPS = 1e-6
NEG_INF = -1e30

kernel_name = "hybrid_dilated_pool_peer_block"


def rmsnorm(x, gain):
    x32 = x.astype(jnp.float32)
    y = x32 * lax.rsqrt(jnp.mean(x32 * x32, axis=-1, keepdims=True) + RMS_EPS)
    return (y * gain.astype(jnp.float32)).astype(x.dtype)


def t5_bucket(rel):
    half = NUM_BUCKETS // 2
    n = -rel
    ret = jnp.where(n < 0, half, 0)
    n = jnp.abs(n)
    max_exact = half // 2
    nf = jnp.maximum(n, 1).astype(jnp.float32)
    large = max_exact + (jnp.log(nf / max_exact) / math.log(MAX_DISTANCE / max_exact)
                         * (half - max_exact)).astype(jnp.int32)
    large = jnp.minimum(large, half - 1)
    return ret + jnp.where(n < max_exact, n, large)


def dilated_window_branch(q, k, v, rel_bias, window, dil):
    B, S, H, Dh = q.shape
    side = window // (2 * dil)
    L = S // dil
    nb = -(-L // side)
    Lp = nb * side

    def to_res(t):
        t = t.reshape(B, L, dil, H, Dh).transpose(0, 2, 3, 1, 4)
        return jnp.pad(t, ((0, 0), (0, 0), (0, 0), (0, Lp - L), (0, 0)))

    def band(t):
        t = jnp.pad(to_res(t), ((0, 0), (0, 0), (0, 0), (side, side), (0, 0)))
        t = t.reshape(B, dil, H, nb + 2, side, Dh)
        return jnp.concatenate([t[:, :, :, :-2], t[:, :, :, 1:-1], t[:, :, :, 2:]], axis=4)

    qb = to_res(q).reshape(B, dil, H, nb, side, Dh)
    kb, vb = band(k), band(v)
    logits = jnp.einsum('brhnqd,brhnkd->brhnqk', qb, kb).astype(jnp.float32) * (Dh ** -0.5)

    t_q = jnp.arange(side)
    t_k = jnp.arange(3 * side)
    rel = t_k[None, :] - side - t_q[:, None]
    bias = rel_bias[t5_bucket(rel * dil)].transpose(2, 0, 1).astype(jnp.float32)
    kj = (jnp.arange(nb)[:, None] - 1) * side + t_k[None, :]
    valid = (jnp.abs(rel) <= side)[None] & ((kj >= 0) & (kj < L))[:, None, :]
    logits = jnp.where(valid[None, None, None], logits + bias[None, None, :, None], NEG_INF)

    lse = jax.nn.logsumexp(logits, axis=-1)
    p = jnp.exp(logits - lse[..., None]).astype(v.dtype)
    o = jnp.einsum('brhnqk,brhnkd->brhnqd', p, vb)
    o = o.reshape(B, dil, H, Lp, Dh)[:, :, :, :L].transpose(0, 3, 1, 2, 4).reshape(B, S, H, Dh)
    lse = lse.reshape(B, dil, H, Lp)[:, :, :, :L].transpose(0, 3, 1, 2).reshape(B, S, H)
    return o, lse


def dilated_attention(q, k, v, rel_bias):
    outs, lses = [], []
    for window, dil in DIL_PAIRS:
        o, lse = dilated_window_branch(q, k, v, rel_bias, window, dil)
        outs.append(o)
        lses.append(lse)
    w = jax.nn.softmax(jnp.stack(lses, axis=0), axis=0).astype(q.dtype)
    return jnp.einsum('gbsh,gbshd->bshd', w, jnp.stack(outs, axis=0))


def multiscale_pool(p, pool_w, pool_scale):
    B, S, C = p.shape
    G = len(POOL_WINDOWS)
    pg = p.reshape(B, S, G, POOL_GROUP).astype(jnp.float32)
    cs = jnp.pad(jnp.cumsum(pg, axis=1), ((0, 0), (1, 0), (0, 0), (0, 0)))
    pos = jnp.arange(S)
    means = []
    for gi, w in enumerate(POOL_WINDOWS):
        lo = jnp.clip(pos - w // 2, 0, S)
        hi = jnp.clip(pos + w // 2, 0, S)
        total = cs[:, hi, gi] - cs[:, lo, gi]
        means.append(total / (hi - lo).astype(jnp.float32)[None, :, None])
    pooled = (jnp.stack(means, axis=2) - pg).astype(p.dtype)
    mixed = jnp.einsum('bsgc,gcd->bsgd', pooled, pool_w)
    return mixed.reshape(B, S, C) * pool_scale


def peer_ffn(h, wq, subkeys, u_tab, v_tab):
    B, S, D = h.shape
    q = (h @ wq).reshape(B, S, PEER_HEADS, 2, PEER_QDIM // 2)
    s = jnp.einsum('bshpd,hpkd->bshpk', q, subkeys).astype(jnp.float32)
    s1, i1 = lax.top_k(s[..., 0, :], PEER_TOPK)
    s2, i2 = lax.top_k(s[..., 1, :], PEER_TOPK)
    cand = (s1[..., :, None] + s2[..., None, :]).reshape(B, S, PEER_HEADS, PEER_TOPK * PEER_TOPK)
    top, ci = lax.top_k(cand, PEER_TOPK)
    e1 = jnp.take_along_axis(i1, ci // PEER_TOPK, axis=-1)
    e2 = jnp.take_along_axis(i2, ci % PEER_TOPK, axis=-1)
    experts = e1 * N_KEYS + e2
    gates = jax.nn.softmax(top, axis=-1).astype(h.dtype)

    T = B * S
    HK = PEER_HEADS * PEER_TOPK
    nc = T // PEER_CHUNK

    def eval_chunk(args):
        hc, ec, gc = args
        a = jnp.einsum('cd,ckd->ck', hc, u_tab[ec])
        act = jax.nn.gelu(a) * gc
        return jnp.einsum('ck,ckd->cd', act, v_tab[ec])

    out = lax.map(eval_chunk, (h.reshape(nc, PEER_CHUNK, D),
                               experts.reshape(nc, PEER_CHUNK, HK),
                               gates.reshape(nc, PEER_CHUNK, HK)))
    return out.reshape(B, S, D)


def setup_inputs(seed: int = 0) -> dict:
    key = jax.random.key(seed)
    ks = jax.random.split(key, 16)
    nrm = jax.random.normal
    f32 = jnp.float32
    x = nrm(ks[0], (BATCH, SEQ, D_MODEL), f32)
    c = nrm(ks[1], (BATCH, D_MODEL), f32)
    w_ada = nrm(ks[2], (DEPTH, D_MODEL, 6 * D_MODEL), f32) * (0.5 * D_MODEL ** -0.5)
    b_ada = nrm(ks[3], (DEPTH, 6 * D_MODEL), f32) * 0.02
    g_mix = 1.0 + 0.05 * nrm(ks[4], (DEPTH, D_MODEL), f32)
    w_in = nrm(ks[5], (DEPTH, D_MODEL, IN_WIDTH), f32) * D_MODEL ** -0.5
    rel_bias = nrm(ks[6], (NUM_BUCKETS, ATT_HEADS), f32) * 0.5
    pool_w = nrm(ks[7], (DEPTH, len(POOL_WINDOWS), POOL_GROUP, POOL_GROUP), f32) * POOL_GROUP ** -0.5
    pool_scale = 1.0 + 0.1 * nrm(ks[8], (DEPTH, POOL_WIDTH), f32)
    w_out = nrm(ks[9], (DEPTH, MIX_WIDTH, D_MODEL), f32) * MIX_WIDTH ** -0.5
    g_ffn = 1.0 + 0.05 * nrm(ks[10], (DEPTH, D_MODEL), f32)
    peer_wq = nrm(ks[11], (DEPTH, D_MODEL, PEER_HEADS * PEER_QDIM), f32) * D_MODEL ** -0.5
    peer_subkeys = nrm(ks[12], (DEPTH, PEER_HEADS, 2, N_KEYS, PEER_QDIM // 2), f32) * (PEER_QDIM // 2) ** -0.5
    peer_u = nrm(ks[13], (DEPTH, N_EXPERTS, D_MODEL), f32) * D_MODEL ** -0.5
    peer_v = nrm(ks[14], (DEPTH, N_EXPERTS, D_MODEL), f32) * PEER_HEADS ** -0.5
    g_final = 1.0 + 0.05 * nrm(ks[15], (D_MODEL,), f32)
    return {"x": x, "c": c, "w_ada": w_ada, "b_ada": b_ada, "g_mix": g_mix, "w_in": w_in,
            "rel_bias": rel_bias, "pool_w": pool_w, "pool_scale": pool_scale, "w_out": w_out,
            "g_ffn": g_ffn, "peer_wq": peer_wq, "peer_subkeys": peer_subkeys,
            "peer_u": peer_u, "peer_v": peer_v, "g_final": g_final}


def reference(x, c, w_ada, b_ada, g_mix, w_in, rel_bias, pool_w, pool_scale, w_out,
              g_ffn, peer_wq, peer_subkeys, peer_u, peer_v, g_final):
    B, S, _ = x.shape
    for l in range(DEPTH):
        mod = jax.nn.silu(c) @ w_ada[l] + b_ada[l]
        sh1, sc1, gt1, sh2, sc2, gt2 = jnp.split(mod, 6, axis=-1)

        h = rmsnorm(x, g_mix[l]) * (1 + sc1[:, None]) + sh1[:, None]
        proj = h @ w_in[l]
        q = proj[..., :ATT_WIDTH].reshape(B, S, ATT_HEADS, HEAD_DIM)
        k = proj[..., ATT_WIDTH:2 * ATT_WIDTH].reshape(B, S, ATT_HEADS, HEAD_DIM)
        v = proj[..., 2 * ATT_WIDTH:3 * ATT_WIDTH].reshape(B, S, ATT_HEADS, HEAD_DIM)
        p_in = proj[..., 3 * ATT_WIDTH:]
        attn = dilated_attention(q, k, v, rel_bias).reshape(B, S, ATT_WIDTH)
        pooled = multiscale_pool(p_in, pool_w[l], pool_scale[l])
        mix = jnp.concatenate([attn, pooled], axis=-1) @ w_out[l]
        x = x + gt1[:, None] * mix

        h2 = rmsnorm(x, g_ffn[l]) * (1 + sc2[:, None]) + sh2[:, None]
        x = x + gt2[:, None] * peer_ffn(h2, peer_wq[l], peer_subkeys[l], peer_u[l], peer_v[l])
    return rmsnorm(x, g_final)
```

```python
import contextlib
import math

import numpy as np

import concourse.bass as bass
import concourse.mybir as mybir
from concourse.bass_utils import run_bass_kernel_spmd

F32 = mybir.dt.float32
BF16 = mybir.dt.bfloat16
U32 = mybir.dt.uint32
AF = mybir.ActivationFunctionType
ALU = mybir.AluOpType
AX = mybir.AxisListType

NEG = -30000.0
DBG_BRANCHES = (0, 1, 2)
DBG_ATT = 9
DBG_EV = 9
RMS_EPS = 1e-6
N_CORES = 8
TOWN = 2048
HALO = 1024
TTOT = 4096
PHALO = 8
PTW = TOWN + 2 * PHALO


class Buf:
    __slots__ = ("name", "w", "wprev", "pw", "pwprev", "r", "rprev", "sem", "cnt", "nobar")

    def __init__(self, name):
        self.name = name
        self.w = None
        self.wprev = None
        self.pw = []
        self.pwprev = []
        self.r = []
        self.rprev = []
        self.sem = None
        self.cnt = 0
        self.nobar = False


class Op:
    __slots__ = ("eng", "fn", "deps", "needs_inc", "token", "dma", "dbuf", "dval")

    def __init__(self, eng, fn, dma):
        self.eng = eng
        self.fn = fn
        self.dma = dma
        self.deps = set()
        self.needs_inc = False
        self.token = 0
        self.dbuf = None
        self.dval = 0


ENGS = ("pe", "act", "dve", "pool", "sp")


class Sched:
    def __init__(self):
        self.ops = {e: [] for e in ENGS}
        self.dma_bufs = []

    def op(self, eng, fn, reads=(), writes=(), pwrites=(), dma=0, sb=None):
        o = Op(eng, fn, dma)
        deps = o.deps
        for b in reads:
            if b.w is not None:
                deps.add(b.w)
            deps.update(b.pw)
        for b in writes:
            if b.w is not None:
                deps.add(b.w)
            deps.update(b.pw)
            deps.update(b.r)
        for b in pwrites:
            if b.r:
                b.rprev = b.r
                b.r = []
                b.wprev = b.w
                b.w = None
                b.pwprev = b.pw
                b.pw = []
            deps.update(b.rprev)
            deps.update(b.pwprev)
            if b.wprev is not None:
                deps.add(b.wprev)
        deps.discard(o)
        for d in deps:
            d.needs_inc = True
        for b in reads:
            b.r.append(o)
        for b in writes:
            b.w = o
            b.pw = []
            b.r = []
            b.rprev = []
            b.pwprev = []
            b.wprev = None
        for b in pwrites:
            b.pw.append(o)
        if dma:
            b = sb if sb is not None else (list(writes) + list(pwrites))[0]
            if b.sem is None:
                b.sem = len(self.dma_bufs)
                self.dma_bufs.append(b)
            b.cnt += 16 * dma
            o.dbuf = b
            o.dval = b.cnt
        self.ops[eng].append(o)
        return o

    def barrier(self, final=False):
        lasts = []
        for e in ENGS:
            for o in reversed(self.ops[e]):
                if not o.dma and o.fn is not None:
                    lasts.append(o)
                    break
        dl = {}
        for e in ENGS:
            for o in self.ops[e]:
                if o.dma and not (o.dbuf.nobar and not final):
                    dl[id(o.dbuf)] = o
        alld = lasts + list(dl.values())
        for e in ENGS:
            o = Op(e, None, 0)
            o.deps = set(alld)
            self.ops[e].append(o)
        for d in alld:
            d.needs_inc = True

    def emit(self, nc):
        for e in ENGS:
            c = 0
            for o in self.ops[e]:
                if not o.dma and o.needs_inc:
                    c += 1
                    o.token = c
        with contextlib.ExitStack() as st:
            esem = {e: st.enter_context(nc.semaphore("s_" + e)) for e in ENGS}
            dsem = [st.enter_context(nc.semaphore("d_%d" % i)) for i in range(len(self.dma_bufs))]
            block = st.enter_context(nc.Block())

            def run(e, eh):
                seen = {}
                for o in self.ops[e]:
                    waits = {}
                    for d in o.deps:
                        if d.dma:
                            key = ("d", d.dbuf.sem)
                            val = d.dval
                        else:
                            if d.eng == "pe" and e == "pe":
                                continue
                            key = ("e", d.eng)
                            val = d.token
                        if waits.get(key, 0) < val:
                            waits[key] = val
                    for key, val in waits.items():
                        if seen.get(key, 0) >= val:
                            continue
                        seen[key] = val
                        sem = dsem[key[1]] if key[0] == "d" else esem[key[1]]
                        eh.wait_ge(sem, val)
                    if o.fn is None:
                        continue
                    if o.dma:
                        o.fn(eh, dsem[o.dbuf.sem])
                    else:
                        ins = o.fn(eh)
                        if o.needs_inc:
                            ins.then_inc(esem[e], 1)

            @block.tensor
            def _(eh):
                run("pe", eh)

            @block.scalar
            def _(eh):
                run("act", eh)

            @block.vector
            def _(eh):
                run("dve", eh)

            @block.gpsimd
            def _(eh):
                run("pool", eh)

            @block.sync
            def _(eh):
                run("sp", eh)


class SBAlloc:
    def __init__(self, nc, base=0, limit=229376):
        self.nc = nc
        self.off = base
        self.limit = limit
        self.n = 0

    def alloc(self, shape, dtype, name=None):
        nbytes = int(np.prod(shape[1:])) * (2 if dtype == BF16 else 4)
        nbytes = (nbytes + 63) // 64 * 64
        self.n += 1
        nm = "%s_%d" % (name or "t", self.n)
        assert self.off + nbytes <= self.limit, (nm, self.off, nbytes)
        t = self.nc.alloc_sbuf_tensor_at(nm, list(shape), dtype, offset=self.off)
        self.off += nbytes
        return t


class Cfg:
    def __init__(self, D=4096, H=16):
        self.D = D
        self.DC = D // 128
        self.H = H
        self.AW = H * 128
        self.PW = D - self.AW
        self.PG = self.PW // 4
        self.PGC = self.PG // 128
        self.PWC = self.PW // 128
        self.MC = D // 128
        self.INW = 3 * self.AW + self.PW
        self.QD = 2048


def build_program(cfg, stop_after=None, debug=False):
    D, DC, H, AW, PW, PG, PGC, PWC, MC, INW = (cfg.D, cfg.DC, cfg.H, cfg.AW, cfg.PW, cfg.PG,
                                               cfg.PGC, cfg.PWC, cfg.MC, cfg.INW)
    nc = bass.Bass("TRN2", target_bir_lowering=False)

    def din(name, shape, dt=F32):
        return nc.dram_tensor(name, list(shape), dt, kind="ExternalInput").ap()

    def dscr(name, shape, dt):
        return nc.dram_tensor(name, list(shape), dt, kind="ExternalOutput" if debug else "Internal").ap()

    xT = din("xT", [D, TTOT])
    vrows = din("vrows", [2, TTOT])
    icnt = din("icnt", [4, TOWN])
    cfm = din("cfm", [128, DC])
    vecs = din("vecs", [128, 6 * DC + 3 * DC + PWC])
    cst = din("cst", [128, 256])
    w_ada = din("w_ada", [D, 6 * D])
    w_in = din("w_in", [D, INW])
    w_out = din("w_out", [D, D])
    w_q = din("w_q", [D, 2048])
    pool_w = din("pool_w", [4, PG, PG])
    skT = din("skT", [128, 16, 128])
    uTt = din("uTt", [128, 128, DC, 128])
    vtt = din("vtt", [DC, 128, 128, 128])
    biasm = din("biasm", [128, H, 3, 256])
    outT = nc.dram_tensor("outT", [D, TOWN], F32, kind="ExternalOutput").ap()

    QT = dscr("QT", [H, 128, TOWN], BF16)
    KT = dscr("KT", [H, 128, TTOT], BF16)
    Vs = dscr("Vs", [TTOT, AW], BF16)
    PT = dscr("PT", [PWC, 128, PTW], BF16)
    mixT = dscr("mixT", [MC, 128, TOWN], BF16)
    X1T = dscr("X1T", [DC, 128, TOWN], F32)
    uTb = nc.dram_tensor("uTb", [128, 128, DC * 128], BF16, kind="Internal").ap()
    vtb = nc.dram_tensor("vtb", [DC, 128, 128 * 128], BF16, kind="Internal").ap()
    wqT = nc.dram_tensor("wqT", [16, 128, DC * 128], BF16, kind="Internal").ap()

    dbg = {}
    if debug:
        dbg["mod"] = nc.dram_tensor("dbg_mod", [128, 6 * DC], F32, kind="ExternalOutput").ap()

    S = Sched()
    b_uTb = Buf("uTb")
    b_vtb = Buf("vtb")
    b_uTb.nobar = True
    b_vtb.nobar = True
    b_wqT = Buf("wqT")
    b_wqT.nobar = True
    PS = nc.alloc_psum_tensor("PS", [128, 8, 512], F32)
    psb = [Buf("psb%d" % i) for i in range(8)]

    CA = SBAlloc(nc, base=16512, limit=16512 + 12 * 1024)
    cst_f = CA.alloc([128, 256], F32, "cst")
    ident_f = cst_f[:, 0:128]
    iota_f = cst_f[:, 128:256]
    ident_b = CA.alloc([128, 128], BF16, "identb")
    ones_b = CA.alloc([128, 128], BF16, "onesb")
    vec_sb = CA.alloc([128, 9 * DC + PWC], F32, "vecs")
    bada = vec_sb[:, 0:6 * DC]
    gmix = vec_sb[:, 6 * DC:7 * DC]
    gffn = vec_sb[:, 7 * DC:8 * DC]
    gfin = vec_sb[:, 8 * DC:9 * DC]
    pscale = vec_sb[:, 9 * DC:9 * DC + PWC]
    mod = CA.alloc([128, 6 * DC], F32, "mod")
    gsc1 = CA.alloc([128, DC], F32, "gsc1")
    gsc2 = CA.alloc([128, DC], F32, "gsc2")
    c_sb = CA.alloc([128, DC], F32, "c")
    cs_b = CA.alloc([128, DC], BF16, "cs")
    skT_b = CA.alloc([128, 16, 128], BF16, "skT")
    sh1, sc1, gt1 = mod[:, 0:DC], mod[:, DC:2 * DC], mod[:, 2 * DC:3 * DC]
    sh2, sc2, gt2 = mod[:, 3 * DC:4 * DC], mod[:, 4 * DC:5 * DC], mod[:, 5 * DC:6 * DC]
    CBASE = 16512 + 12 * 1024
    b_const = Buf("const")
    b_const2 = Buf("const2")
    b_mod = Buf("mod")

    def dma1(out, in_, **kw):
        def fn(eh, sem):
            eh.dma_start(out=out, in_=in_, **kw).then_inc(sem, 16)
        return fn

    def dman(pairs, **kw):
        def fn(eh, sem):
            for (o, i) in pairs:
                eh.dma_start(out=o, in_=i, **kw).then_inc(sem, 16)
        return fn

    S.op("sp", dman([(cst_f[:], cst[:, :]), (vec_sb[:], vecs[:, :]), (c_sb[:], cfm[:, :])]),
         writes=[b_const], dma=3)
    S.op("pool", dma1(skT_b[:], skT[:, :, :]), writes=[b_const2], dma=1)
    S.op("dve", lambda e: e.tensor_copy(out=ident_b[:], in_=ident_f), reads=[b_const], writes=[Buf("identb")])
    b_cst3 = Buf("const3")
    S.op("dve", lambda e: e.memset(ones_b[:], 1.0), writes=[b_cst3])
    b_cs = Buf("cs")
    S.op("act", lambda e: e.activation(out=cs_b[:], in_=c_sb[:], func=AF.Silu), reads=[b_const], writes=[b_cs])

    rr = [0]

    def evac_eng():
        rr[0] += 1
        return "act" if rr[0] % 2 else "dve"

    def copy_fn(eng, out, in_):
        if eng == "act":
            return lambda e: e.copy(out=out, in_=in_)
        return lambda e: e.tensor_copy(out=out, in_=in_)

    A = SBAlloc(nc, base=CBASE)
    NB = 512
    nblk = 6 * D // NB
    wa = [A.alloc([128, DC, NB], BF16, "wa") for _ in range(2)]
    b_wa = [Buf("wa%d" % i) for i in range(2)]
    modrow = A.alloc([1, 6 * D], F32, "modrow")
    b_modrow = Buf("modrow")
    w_ada_v = w_ada.rearrange("(dc p) n -> p dc n", p=128)
    hdc = max(1, DC // 2)
    for nb in range(nblk):
        sl = nb % 2
        pairs = []
        for q in range(0, DC, hdc):
            pairs.append((wa[sl][:, q:q + hdc, :], w_ada_v[:, q:q + hdc, nb * NB:(nb + 1) * NB]))
        S.op("pool", dman(pairs), writes=[b_wa[sl]], dma=len(pairs))
        pb = 1 + nb % 2

        def mmA(e, sl=sl, pb=pb):
            for dc in range(DC):
                ins = e.matmul(PS[0:1, pb, :], lhsT=cs_b[:, dc:dc + 1], rhs=wa[sl][:, dc, :],
                               start=(dc == 0), stop=(dc == DC - 1))
            return ins
        S.op("pe", mmA, reads=[b_wa[sl], b_cs], writes=[psb[pb]])
        eng = evac_eng()
        S.op(eng, copy_fn(eng, modrow[0:1, nb * NB:(nb + 1) * NB], PS[0:1, pb, :]),
             reads=[psb[pb]], pwrites=[b_modrow])

    def mmT(e):
        for jj in range(6 * DC):
            ins = e.matmul(PS[:, 3, jj:jj + 1], lhsT=modrow[0:1, jj * 128:(jj + 1) * 128],
                           rhs=ident_f[0:1, 0:1], start=True, stop=True)
        return ins
    S.op("pe", mmT, reads=[b_modrow, b_const], writes=[psb[3]])
    S.op("dve", lambda e: e.tensor_tensor(out=mod[:], in0=PS[:, 3, 0:6 * DC], in1=bada, op=ALU.add),
         reads=[psb[3], b_const], writes=[b_mod])
    S.op("dve", lambda e: e.scalar_tensor_tensor(out=gsc1[:], in0=sc1, scalar=1.0, in1=gmix,
                                                 op0=ALU.add, op1=ALU.mult),
         reads=[b_mod], writes=[Buf("gsc1")])
    b_gsc = Buf("gsc")
    S.op("dve", lambda e: e.scalar_tensor_tensor(out=gsc2[:], in0=sc2, scalar=1.0, in1=gffn,
                                                 op0=ALU.add, op1=ALU.mult),
         reads=[b_mod], writes=[b_gsc])
    if debug:
        b_dbg = Buf("dbg")
        S.op("sp", dma1(dbg["mod"][:, :], mod[:]), reads=[b_mod, b_gsc], writes=[b_dbg], dma=1)
    S.barrier()
    if stop_after == "A":
        return finish(nc, S, outT, None)

    def norm_tile(load_chunk, NT, hT, b_hT_chunks, gsc, sh, ss_bank, tmps, sqs, rst, b_tmps, b_sqs, b_rst,
                  xbufs=None):
        nsq = len(sqs)
        for dc in range(DC):
            xa, xb = load_chunk(dc, 0)
            k = dc % nsq
            S.op("act", lambda e, xa=xa, k=k: e.activation(out=sqs[k][:, 0:NT], in_=xa, func=AF.Square),
                 reads=[xb], writes=[b_sqs[k]])
            S.op("pe", lambda e, k=k, dc=dc: e.matmul(PS[:, ss_bank, 0:NT], lhsT=ones_b[:], rhs=sqs[k][:, 0:NT],
                                                      start=(dc == 0), stop=(dc == DC - 1)),
                 reads=[b_sqs[k], b_cst3], writes=[psb[ss_bank]] if dc == 0 else [], pwrites=[] if dc == 0 else [psb[ss_bank]])
        S.op("dve", lambda e: e.tensor_scalar(out=rst[0][:, 0:NT], in0=PS[:, ss_bank, 0:NT], scalar1=1.0 / D,
                                              scalar2=RMS_EPS, op0=ALU.mult, op1=ALU.add),
             reads=[psb[ss_bank]], writes=[b_rst[0]])
        S.op("act", lambda e: e.activation(out=rst[1][:, 0:NT], in_=rst[0][:, 0:NT], func=AF.Sqrt),
             reads=[b_rst[0]], writes=[b_rst[1]])
        S.op("dve", lambda e: e.reciprocal(out=rst[0][:, 0:NT], in_=rst[1][:, 0:NT]),
             reads=[b_rst[1]], writes=[b_rst[0]])
        nt = len(tmps)
        for dc in range(DC):
            xa, xb = load_chunk(dc, 1)
            k = dc % nt
            S.op("dve", lambda e, xa=xa, k=k, dc=dc: e.scalar_tensor_tensor(
                out=tmps[k][:, 0:NT], in0=xa, scalar=gsc[:, dc:dc + 1], in1=rst[0][:, 0:NT],
                op0=ALU.mult, op1=ALU.mult),
                reads=[xb, b_rst[0], b_gsc], writes=[b_tmps[k]])
            S.op("act", lambda e, k=k, dc=dc: e.activation(out=hT[:, dc, 0:NT], in_=tmps[k][:, 0:NT],
                                                           func=AF.Identity, bias=sh[:, dc:dc + 1]),
                 reads=[b_tmps[k], b_mod], writes=[b_hT_chunks[dc]])

    A = SBAlloc(nc, base=CBASE)
    NT1 = 512
    WB = 256
    xt = A.alloc([128, DC, NT1], F32, "xt")
    b_xt = Buf("xt")
    hTs = [A.alloc([128, DC, NT1], BF16, "hT") for _ in range(2)]
    b_hT = [[Buf("hT%d_%d" % (s, dc)) for dc in range(DC)] for s in range(2)]
    wbs = [A.alloc([128, DC, WB], BF16, "wb") for _ in range(2)]
    b_wb = [Buf("wb%d" % i) for i in range(2)]
    sqs = [A.alloc([128, NT1], BF16, "sq") for _ in range(4)]
    b_sqs = [Buf("sq%d" % i) for i in range(4)]
    tmps = [A.alloc([128, NT1], F32, "tmp") for _ in range(3)]
    b_tmps = [Buf("tmp%d" % i) for i in range(3)]
    rst = [A.alloc([128, NT1], F32, "rst") for _ in range(2)]
    b_rst = [Buf("rst%d" % i) for i in range(2)]
    stg = [A.alloc([128, NT1], BF16, "stg") for _ in range(4)]
    b_stg = [Buf("stg%d" % i) for i in range(4)]
    b_QT, b_KT, b_Vs, b_PT = Buf("QT"), Buf("KT"), Buf("Vs"), Buf("PT")
    xT_v = xT.rearrange("(dc p) t -> p dc t", p=128)
    w_in_v = w_in.rearrange("(dc p) n -> p dc n", p=128)
    ntiles = TTOT // NT1
    cnt = {"w": 0, "ps": 0, "stg": 0}

    def load_x(i):
        pairs = []
        q4 = max(1, DC // 4)
        for q in range(0, DC, q4):
            pairs.append((xt[:, q:q + q4, :], xT_v[:, q:q + q4, i * NT1:(i + 1) * NT1]))
        S.op("sp", dman(pairs), writes=[b_xt], dma=len(pairs))

    def norm1(i):
        s = i % 2
        norm_tile(lambda dc, p: (xt[:, dc, :], b_xt), NT1, hTs[s], b_hT[s], gsc1, sh1, 0,
                  tmps, sqs, rst, b_tmps, b_sqs, b_rst)

    def proj1(i):
        s = i % 2
        own = 2 <= i <= 5
        hT = hTs[s]
        for cb in range(INW // WB):
            c0 = cb * WB
            kind = "q" if c0 < AW else "k" if c0 < 2 * AW else "v" if c0 < 3 * AW else "p"
            tok = None
            if kind in ("q",) and not own:
                continue
            if kind == "p" and not own:
                if i == 1:
                    tok = (NT1 - PHALO, NT1, 0)
                elif i == 6:
                    tok = (0, PHALO, PHALO + TOWN)
                else:
                    continue
            sl = cnt["w"] % 2
            cnt["w"] += 1
            pairs = []
            for q in range(0, DC, hdc):
                pairs.append((wbs[sl][:, q:q + hdc, :], w_in_v[:, q:q + hdc, c0:c0 + WB]))
            S.op("pool", dman(pairs), writes=[b_wb[sl]], dma=len(pairs))
            if kind == "v":
                for ts in range(NT1 // 128):
                    pb = 1 + cnt["ps"] % 4
                    cnt["ps"] += 1

                    def mmv(e, sl=sl, pb=pb, ts=ts, hT=hT):
                        for dc in range(DC):
                            ins = e.matmul(PS[:, pb, 0:WB], lhsT=hT[:, dc, ts * 128:(ts + 1) * 128],
                                           rhs=wbs[sl][:, dc, :], start=(dc == 0), stop=(dc == DC - 1))
                        return ins
                    S.op("pe", mmv, reads=[b_wb[sl]] + b_hT[s], writes=[psb[pb]])
                    k = cnt["stg"] % 4
                    cnt["stg"] += 1
                    eng = evac_eng()
                    S.op(eng, copy_fn(eng, stg[k][:, 0:WB], PS[:, pb, 0:WB]), reads=[psb[pb]], writes=[b_stg[k]])
                    r0 = i * NT1 + ts * 128
                    S.op("sp", dma1(Vs[r0:r0 + 128, c0 - 2 * AW:c0 - 2 * AW + WB], stg[k][:, 0:WB]),
                         reads=[b_stg[k]], pwrites=[b_Vs], dma=1, sb=b_stg[k])
            else:
                for nt in range(WB // 128):
                    pb = 1 + cnt["ps"] % 4
                    cnt["ps"] += 1
                    t0, t1 = (0, NT1) if tok is None else (tok[0], tok[1])
                    n = t1 - t0

                    def mmf(e, sl=sl, pb=pb, nt=nt, hT=hT, t0=t0, t1=t1, n=n):
                        for dc in range(DC):
                            ins = e.matmul(PS[:, pb, 0:n], lhsT=wbs[sl][:, dc, nt * 128:(nt + 1) * 128],
                                           rhs=hT[:, dc, t0:t1], start=(dc == 0), stop=(dc == DC - 1))
                        return ins
                    S.op("pe", mmf, reads=[b_wb[sl]] + b_hT[s], writes=[psb[pb]])
                    k = cnt["stg"] % 4
                    cnt["stg"] += 1
                    eng = evac_eng()
                    S.op(eng, copy_fn(eng, stg[k][:, 0:n], PS[:, pb, 0:n]), reads=[psb[pb]], writes=[b_stg[k]])
                    col = c0 + nt * 128
                    if kind == "q":
                        hh = col // 128
                        dst, bb = QT[hh][:, (i - 2) * NT1:(i - 1) * NT1], b_QT
                    elif kind == "k":
                        hh = (col - AW) // 128
                        dst, bb = KT[hh][:, i * NT1:(i + 1) * NT1], b_KT
                    else:
                        ch = (col - 3 * AW) // 128
                        if tok is None:
                            dst = PT[ch][:, PHALO + (i - 2) * NT1:PHALO + (i - 1) * NT1]
                        else:
                            dst = PT[ch][:, tok[2]:tok[2] + PHALO]
                        bb = b_PT
                    S.op("sp", dma1(dst, stg[k][:, 0:n]), reads=[b_stg[k]], pwrites=[bb], dma=1, sb=b_stg[k])

    load_x(0)
    norm1(0)
    for i in range(ntiles):
        if i + 1 < ntiles:
            load_x(i + 1)
            norm1(i + 1)
        proj1(i)
    S.barrier()
    if stop_after == "1":
        return finish(nc, S, outT, None)

    A = SBAlloc(nc, base=CBASE)
    scale = 128.0 ** -0.5
    KTh = [A.alloc([128, TTOT], BF16, "KTh") for _ in range(2)]
    QTh = [A.alloc([128, TOWN], BF16, "QTh") for _ in range(2)]
    V1 = [A.alloc([128, 17, 128], BF16, "V1") for _ in range(2)]
    V4 = [A.alloc([128, 4, 5, 128], BF16, "V4") for _ in range(2)]
    V16 = [A.alloc([128, 16, 2, 128], BF16, "V16") for _ in range(2)]
    bia = [A.alloc([128, 3, 256], F32, "bias") for _ in range(2)]
    b_hd = [Buf("hd%d" % i) for i in range(2)]
    num = [A.alloc([128, TOWN], F32, "num") for _ in range(2)]
    den = [A.alloc([128, TOWN], F32, "den") for _ in range(2)]
    b_num = [Buf("num%d" % i) for i in range(2)]
    b_den = [Buf("den%d" % i) for i in range(2)]
    vneg = A.alloc([128, TTOT], F32, "vneg")
    b_vneg = Buf("vneg")
    NSL = 4
    Ls = [A.alloc([128, 256], F32, "L") for _ in range(NSL)]
    b_L = [Buf("L%d" % i) for i in range(NSL)]
    Pb = [A.alloc([128, 256], BF16, "P") for _ in range(NSL)]
    b_P = [Buf("P%d" % i) for i in range(NSL)]
    PTs = [A.alloc([128, 256], BF16, "PTs") for _ in range(NSL)]
    b_PTs = [Buf("PTs%d" % i) for i in range(NSL)]
    ast = [A.alloc([128, TOWN], BF16, "ast") for _ in range(2)]
    b_ast = [Buf("ast%d" % i) for i in range(2)]
    b_mixT = Buf("mixT")
    PSb = PS[:, :, :].bitcast(BF16)

    S.op("sp", dma1(vneg[:], vrows[1:2, :].partition_broadcast(128).rearrange("p o t -> p (o t)")), writes=[b_vneg], dma=1)
    uT_src = uTt.rearrange("c p dc e -> (c p) (dc e)").rearrange("r (a k) -> (r a) k", k=min(2048, DC * 128))
    uT_dst = uTb.rearrange("c p n -> (c p) n").rearrange("r (a k) -> (r a) k", k=min(2048, DC * 128))
    v_src = vtt.rearrange("j p c d -> (j p) (c d)").rearrange("r (a k) -> (r a) k", k=2048)
    v_dst = vtb.rearrange("j p n -> (j p) n").rearrange("r (a k) -> (r a) k", k=2048)
    pre_pieces = []
    stepu = 1024
    for r0 in range(0, uT_src.shape[0], stepu):
        pre_pieces.append((uT_dst[r0:r0 + stepu, :], uT_src[r0:r0 + stepu, :], b_uTb))
    for r0 in range(0, v_src.shape[0], stepu):
        pre_pieces.append((v_dst[r0:r0 + stepu, :], v_src[r0:r0 + stepu, :], b_vtb))
    wq_src = w_q.rearrange("(dc p) (hp e) -> hp p dc e", p=128, e=128)
    for hp in range(16):
        pre_pieces.append((wqT[hp].rearrange("p (dc e) -> p dc e", e=128), wq_src[hp], b_wqT))
    pre_state = {"i": 0}

    def emit_pre(upto):
        while pre_state["i"] < min(upto, len(pre_pieces)):
            d_, s_, b_ = pre_pieces[pre_state["i"]]
            pre_state["i"] += 1
            S.op("pool", dma1(d_, s_), pwrites=[b_], dma=1, sb=b_)

    def load_head(h):
        s = h % 2
        hc = slice(h * 128, (h + 1) * 128)
        pairs = [
            (KTh[s][:], KT[h]),
            (QTh[s][:], QT[h]),
            (V1[s][:], Vs[960:960 + 17 * 128, hc].rearrange("(kc p) d -> p kc d", p=128)),
            (bia[s][:], biasm[:, h]),
        ]
        for r in range(4):
            pairs.append((V4[s][:, r], Vs[768:768 + 5 * 512, hc].rearrange("(kc p r) d -> p r kc d", p=128, r=4)[:, r]))
        for r in range(16):
            pairs.append((V16[s][:, r], Vs[0:4096, hc].rearrange("(kc p r) d -> p r kc d", p=128, r=16)[:, r]))
        S.op("sp", dman(pairs), reads=[b_QT, b_KT, b_Vs], writes=[b_hd[s]], dma=len(pairs))

    def finish_head(h):
        s = h % 2
        S.op("dve", lambda e, s=s: e.reciprocal(out=den[s][:], in_=den[s][:]), reads=[b_den[s]], writes=[b_den[s]])
        S.op("dve", lambda e, s=s: e.tensor_tensor(out=ast[s][:], in0=num[s][:], in1=den[s][:], op=ALU.mult),
             reads=[b_den[s], b_num[s]], writes=[b_ast[s]])
        S.op("sp", dma1(mixT[h], ast[s][:]), reads=[b_ast[s]], pwrites=[b_mixT], dma=1, sb=b_ast[s])
        if h + 2 < H:
            load_head(h + 2)

    tiles = []
    for h in range(H):
        first = True
        for br, dil in enumerate((1, 4, 16)):
            for r in range(dil):
                for m in range(16 // dil):
                    tiles.append(dict(h=h, br=br, dil=dil, r=r, m=m, q=len(tiles), last=False))
        tiles[-1]["last"] = True

    def st0(t):
        h, br, dil, r, m, q = t["h"], t["br"], t["dil"], t["r"], t["m"], t["q"]
        s = h % 2
        k4 = q % NSL
        sb_ = q % 2
        qs = r + dil * 128 * m
        ks = r + HALO + dil * (128 * m - 64)
        q_ap = QTh[s][:, qs:qs + dil * 127 + 1:dil]
        k_ap = KTh[s][:, ks:ks + dil * 255 + 1:dil]
        vn_ap = vneg[:, ks:ks + dil * 255 + 1:dil]
        S_ps = PS[:, sb_, 0:256]
        S.op("pe", lambda e: e.matmul(S_ps, lhsT=q_ap, rhs=k_ap, start=True, stop=True),
             reads=[b_hd[s]], writes=[psb[sb_]])
        S.op("dve", lambda e: e.scalar_tensor_tensor(out=Ls[k4][:], in0=S_ps, scalar=scale, in1=bia[s][:, br, :],
                                                     op0=ALU.mult, op1=ALU.add),
             reads=[psb[sb_], b_hd[s]], writes=[b_L[k4]])
        S.op("pool", lambda e: e.tensor_tensor(out=Ls[k4][:], in0=Ls[k4][:], in1=vn_ap, op=ALU.add),
             reads=[b_vneg, b_L[k4]], writes=[b_L[k4]])
        S.op("act", lambda e: e.activation(out=Pb[k4][:], in_=Ls[k4][:], func=AF.Exp),
             reads=[b_L[k4]], writes=[b_P[k4]])

    def st1(t):
        q = t["q"]
        k4 = q % NSL
        tb = 2 + q % 2
        PT_ps = PSb[:, tb, 0:256]

        def trf(e):
            e.transpose(PT_ps[:, 0:128], Pb[k4][:, 0:128], ident_b[:])
            return e.transpose(PT_ps[:, 128:256], Pb[k4][:, 128:256], ident_b[:])
        S.op("pe", trf, reads=[b_P[k4]], writes=[psb[tb]])
        S.op("act", lambda e: e.copy(out=PTs[k4][:], in_=PT_ps), reads=[psb[tb]], writes=[b_PTs[k4]])

    def st2(t):
        h, br, dil, r, m, q = t["h"], t["br"], t["dil"], t["r"], t["m"], t["q"]
        s = h % 2
        k4 = q % NSL
        ob = 4 + q % 2
        qs = r + dil * 128 * m
        if dil == 1:
            v0, v1 = V1[s][:, m, :], V1[s][:, m + 1, :]
        elif dil == 4:
            v0, v1 = V4[s][:, r, m, :], V4[s][:, r, m + 1, :]
        else:
            v0, v1 = V16[s][:, r, 0, :], V16[s][:, r, 1, :]

        def pvf(e):
            e.matmul(PS[:, ob, 0:128], lhsT=v0, rhs=PTs[k4][:, 0:128], start=True, stop=False)
            e.matmul(PS[:, ob, 0:128], lhsT=v1, rhs=PTs[k4][:, 128:256], start=False, stop=True)
            e.matmul(PS[:, ob + 2, 0:128], lhsT=ones_b[:], rhs=PTs[k4][:, 0:128], start=True, stop=False)
            return e.matmul(PS[:, ob + 2, 0:128], lhsT=ones_b[:], rhs=PTs[k4][:, 128:256], start=False, stop=True)
        S.op("pe", pvf, reads=[b_PTs[k4], b_hd[s], b_cst3], writes=[psb[ob], psb[ob + 2]])
        n_ap = num[s][:, qs:qs + dil * 127 + 1:dil]
        d_ap = den[s][:, qs:qs + dil * 127 + 1:dil]
        if br == 0:
            S.op("dve", lambda e: e.tensor_copy(out=n_ap, in_=PS[:, ob, 0:128]), reads=[psb[ob]], pwrites=[b_num[s]])
            S.op("dve", lambda e: e.tensor_copy(out=d_ap, in_=PS[:, ob + 2, 0:128]), reads=[psb[ob + 2]],
                 pwrites=[b_den[s]])
        else:
            S.op("dve", lambda e: e.tensor_tensor(out=n_ap, in0=PS[:, ob, 0:128], in1=n_ap, op=ALU.add),
                 reads=[psb[ob], b_num[s]], pwrites=[b_num[s]])
            S.op("dve", lambda e: e.tensor_tensor(out=d_ap, in0=PS[:, ob + 2, 0:128], in1=d_ap, op=ALU.add),
                 reads=[psb[ob + 2], b_den[s]], pwrites=[b_den[s]])
        if t["last"]:
            finish_head(h)

    load_head(0)
    if H > 1:
        load_head(1)
    nt_ = len(tiles)
    for i in range(nt_ + 2):
        emit_pre((i + 1) * len(pre_pieces) // max(1, nt_ - 8) + 1)
        if i < nt_:
            st0(tiles[i])
        if 0 <= i - 1 < nt_:
            st1(tiles[i - 1])
        if 0 <= i - 2 < nt_:
            st2(tiles[i - 2])
    S.barrier()
    if stop_after == "2":
        return finish(nc, S, outT, None)

    A = SBAlloc(nc, base=CBASE)
    v01 = A.alloc([128, PTW], F32, "v01")
    b_v01 = Buf("v01")
    icn = [A.alloc([128, TOWN], F32, "icn") for _ in range(2)]
    b_icn = [Buf("icn%d" % i) for i in range(2)]
    ptl = [A.alloc([128, PTW], BF16, "ptl") for _ in range(2)]
    b_ptl = [Buf("ptl%d" % i) for i in range(2)]
    pm = [A.alloc([128, PTW], F32, "pm") for _ in range(2)]
    sA = [A.alloc([128, PTW], F32, "sA") for _ in range(2)]
    sB = [A.alloc([128, PTW], F32, "sB") for _ in range(2)]
    b_pl = [Buf("pl%d" % i) for i in range(2)]
    pooled = [A.alloc([128, PGC, TOWN], BF16, "pooled") for _ in range(2)]
    b_pooled = [[Buf("pooled%d_%d" % (i, c)) for c in range(PGC)] for i in range(2)]
    wp = [A.alloc([128, PGC, PG], BF16, "wp") for _ in range(2)]
    b_wp = [Buf("wp%d" % i) for i in range(2)]
    stg3 = [A.alloc([128, 512], BF16, "stg3") for _ in range(3)]
    b_stg3 = [Buf("stg3_%d" % i) for i in range(3)]
    S.op("sp", dma1(v01[:], vrows[0:1, HALO - PHALO:HALO + TOWN + PHALO].partition_broadcast(128).rearrange("p o t -> p (o t)")),
         writes=[b_v01], dma=1)
    c3 = {"ps": 0, "stg": 0, "ch": 0}
    for g in range(4):
        gs = g % 2
        S.op("sp", dma1(icn[gs][:], icnt[g:g + 1, :].partition_broadcast(128).rearrange("p o t -> p (o t)")), writes=[b_icn[gs]], dma=1)
        S.op("pool", dma1(wp[gs][:], pool_w[g].rearrange("(kc p) n -> p kc n", p=128)), writes=[b_wp[gs]], dma=1)
        for pc in range(PGC):
            ch = g * PGC + pc
            k = c3["ch"] % 2
            c3["ch"] += 1
            eng = "dve" if k == 0 else "pool"
            S.op("sp", dma1(ptl[k][:], PT[ch]), reads=[b_PT], writes=[b_ptl[k]], dma=1)
            E = PTW

            def tt(out, a, b, op):
                return lambda e: e.tensor_tensor(out=out, in0=a, in1=b, op=op)
            S.op(eng, tt(pm[k][:], ptl[k][:], v01[:], ALU.mult), reads=[b_ptl[k], b_v01], writes=[b_pl[k]])
            S.op(eng, tt(sA[k][:, 1:E], pm[k][:, 1:E], pm[k][:, 0:E - 1], ALU.add), reads=[b_pl[k]], writes=[b_pl[k]])
            cur, oth = sA[k], sB[k]
            lo, hi, sh_ = 1, E, 1
            for lvl in range(g):
                nlo, nhi = lo + sh_, hi - sh_
                S.op(eng, tt(oth[:, nlo:nhi], cur[:, nlo + sh_:nhi + sh_], cur[:, nlo - sh_:nhi - sh_], ALU.add),
                     reads=[b_pl[k]], writes=[b_pl[k]])
                cur, oth = oth, cur
                lo, hi = nlo, nhi
                sh_ *= 2
            assert lo <= PHALO and hi >= PHALO + TOWN, (lo, hi)
            S.op(eng, tt(oth[:, 0:TOWN], cur[:, PHALO:PHALO + TOWN], icn[gs][:], ALU.mult),
                 reads=[b_pl[k], b_icn[gs]], writes=[b_pl[k]])
            S.op(eng, tt(pooled[gs][:, pc, :], oth[:, 0:TOWN], pm[k][:, PHALO:PHALO + TOWN], ALU.subtract),
                 reads=[b_pl[k]], writes=[b_pooled[gs][pc]])
        for jo in range(PGC):
            for tb in range(TOWN // 512):
                pb = 1 + c3["ps"] % 4
                c3["ps"] += 1

                def mmp(e, gs=gs, jo=jo, tb=tb, pb=pb):
                    for kc in range(PGC):
                        ins = e.matmul(PS[:, pb, :], lhsT=wp[gs][:, kc, jo * 128:(jo + 1) * 128],
                                       rhs=pooled[gs][:, kc, tb * 512:(tb + 1) * 512],
                                       start=(kc == 0), stop=(kc == PGC - 1))
                    return ins
                S.op("pe", mmp, reads=[b_wp[gs]] + b_pooled[gs], writes=[psb[pb]])
                k = c3["stg"] % 3
                c3["stg"] += 1
                col = g * PGC + jo
                S.op("act", lambda e, k=k, pb=pb, col=col: e.activation(out=stg3[k][:], in_=PS[:, pb, :],
                                                                        func=AF.Identity,
                                                                        scale=pscale[:, col:col + 1]),
                     reads=[psb[pb], b_const], writes=[b_stg3[k]])
                S.op("sp", dma1(mixT[H + col][:, tb * 512:(tb + 1) * 512], stg3[k][:]),
                     reads=[b_stg3[k]], pwrites=[b_mixT], dma=1, sb=b_stg3[k])
    S.barrier()

    A = SBAlloc(nc, base=CBASE)
    TB2 = 1024
    mxl = A.alloc([128, MC, TB2], BF16, "mxl")
    b_mxl = Buf("mxl")
    wo = [A.alloc([128, MC, WB], BF16, "wo") for _ in range(2)]
    b_wo = [Buf("wo%d" % i) for i in range(2)]
    xo = [A.alloc([128, 512], F32, "xo") for _ in range(4)]
    b_xo = [Buf("xo%d" % i) for i in range(4)]
    x1s = [A.alloc([128, 512], F32, "x1s") for _ in range(4)]
    b_x1s = [Buf("x1s%d" % i) for i in range(4)]
    b_X1T = Buf("X1T")
    mixT_v = mixT.rearrange("c p t -> p c t")
    w_out_v = w_out.rearrange("(mc p) n -> p mc n", p=128)
    c4 = {"w": 0, "ps": 0, "x": 0}
    for tb in range(TOWN // TB2):
        pairs = []
        q4 = max(1, MC // 4)
        for q in range(0, MC, q4):
            pairs.append((mxl[:, q:q + q4, :], mixT_v[:, q:q + q4, tb * TB2:(tb + 1) * TB2]))
        S.op("sp", dman(pairs), reads=[b_mixT], writes=[b_mxl], dma=len(pairs))
        for cb in range(D // WB):
            sl = c4["w"] % 2
            c4["w"] += 1
            pairs = []
            for q in range(0, MC, hdc):
                pairs.append((wo[sl][:, q:q + hdc, :], w_out_v[:, q:q + hdc, cb * WB:(cb + 1) * WB]))
            S.op("pool", dman(pairs), writes=[b_wo[sl]], dma=len(pairs))
            for nt in range(WB // 128):
                j = cb * (WB // 128) + nt
                for hf in range(TB2 // 512):
                    t0 = tb * TB2 + hf * 512
                    pb = 1 + c4["ps"] % 4
                    c4["ps"] += 1
                    k = c4["x"] % 4
                    c4["x"] += 1
                    S.op("sp", dma1(xo[k][:], xT[j * 128:(j + 1) * 128, HALO + t0:HALO + t0 + 512]),
                         writes=[b_xo[k]], dma=1)

                    def mmo(e, sl=sl, nt=nt, hf=hf, pb=pb):
                        for mc in range(MC):
                            ins = e.matmul(PS[:, pb, :], lhsT=wo[sl][:, mc, nt * 128:(nt + 1) * 128],
                                           rhs=mxl[:, mc, hf * 512:(hf + 1) * 512], start=(mc == 0), stop=(mc == MC - 1))
                        return ins
                    S.op("pe", mmo, reads=[b_wo[sl], b_mxl], writes=[psb[pb]])
                    S.op("dve", lambda e, k=k, pb=pb, j=j: e.scalar_tensor_tensor(
                        out=x1s[k][:], in0=PS[:, pb, :], scalar=gt1[:, j:j + 1], in1=xo[k][:],
                        op0=ALU.mult, op1=ALU.add),
                        reads=[psb[pb], b_xo[k], b_mod], writes=[b_x1s[k]])
                    S.op("sp", dma1(X1T[j][:, t0:t0 + 512], x1s[k][:]), reads=[b_x1s[k]], pwrites=[b_X1T], dma=1,
                         sb=b_x1s[k])
    S.barrier()
    if stop_after == "3":
        return finish(nc, S, outT, None)

    A = SBAlloc(nc, base=CBASE)
    T4 = 256
    NSUB = T4 // 128
    G = A.alloc([128, 128, T4], BF16, "G")
    b_G = [Buf("G%d" % c) for c in range(128)]
    b_Gall = Buf("Gall")
    stage_base = A.off
    vS = [A.alloc([128, 64, 128], BF16, "vS") for _ in range(3)]
    b_vS = [Buf("vS%d" % i) for i in range(3)]
    after_stage = A.off
    A.off = stage_base
    h2T = A.alloc([128, DC, T4], BF16, "h2T")
    b_h2T = [Buf("h2T_%d" % dc) for dc in range(DC)]
    nu = 4
    uS = [A.alloc([128, DC, 128], BF16, "uS") for _ in range(nu)]
    b_uS = [Buf("uS%d" % i) for i in range(nu)]
    wq_s = uS
    assert A.off <= after_stage, (A.off, after_stage)
    b_stage = Buf("stage")
    A.off = after_stage
    wqb = [A.alloc([128, DC, 128], BF16, "wqb") for _ in range(2)]
    b_wqb = [Buf("wqb%d" % i) for i in range(2)]
    qT = A.alloc([128, 16, T4], BF16, "qT")
    b_qT = [Buf("qT%d" % i) for i in range(16)]
    sc = A.alloc([128, 16, 128], F32, "sc")
    b_sc = Buf("sc")
    scrs = [A.alloc([128, 256], F32, "scr") for _ in range(4)]
    b_scrs = [Buf("scr%d" % i) for i in range(4)]
    b_tkh = [Buf("tkh%d" % i) for i in range(16)]
    b_toph = [Buf("toph%d" % i) for i in range(8)]
    tv = A.alloc([128, 8, 2, 16], F32, "tv")
    ti = A.alloc([128, 8, 2, 16], U32, "ti")
    tif = A.alloc([128, 8, 2, 16], F32, "tif")
    b_tk = Buf("tk")
    cand = A.alloc([128, 8, 16, 16], F32, "cand")
    b_cand = Buf("cand")
    topv = A.alloc([128, 8, 16], F32, "topv")
    ci = A.alloc([128, 8, 16], U32, "ci")
    ca_i = A.alloc([128, 8, 16], U32, "ca_i")
    cb_i = A.alloc([128, 8, 16], U32, "cb_i")
    caf = A.alloc([128, 8, 16], F32, "caf")
    cbf = A.alloc([128, 8, 16], F32, "cbf")
    oh = sc[:].rearrange("p a k -> p (a k)").rearrange("p (h a b) -> p h a b", h=8, a=16)
    b_oh = b_sc
    egt = A.alloc([128, 3, 128], F32, "egt")
    b_egt = Buf("egt")
    gsm = A.alloc([128, 16], F32, "gsm")
    dummy = A.alloc([128, 2], F32, "dummy")
    eT = A.alloc([128, 3, T4], F32, "eT")
    b_eT = Buf("eT")
    NG = 8
    A16 = [A.alloc([128, NG, 128], BF16, "A16") for _ in range(2)]
    B16 = [A.alloc([128, NG, 128], BF16, "B16") for _ in range(2)]
    b_AB = [Buf("AB%d" % i) for i in range(2)]
    x1c = [A.alloc([128, T4], F32, "x1c") for _ in range(4)]
    b_x1c = [Buf("x1c%d" % i) for i in range(4)]
    sqs4 = [A.alloc([128, T4], BF16, "sq4") for _ in range(3)]
    b_sqs4 = [Buf("sq4_%d" % i) for i in range(3)]
    tmps4 = [A.alloc([128, T4], F32, "tmp4") for _ in range(3)]
    b_tmps4 = [Buf("tmp4_%d" % i) for i in range(3)]
    rst4 = [A.alloc([128, T4], F32, "rst4") for _ in range(2)]
    b_rst4 = [Buf("rst4_%d" % i) for i in range(2)]
    gl = [A.alloc([128, T4], F32, "gl") for _ in range(2)]
    b_gl = [Buf("gl%d" % i) for i in range(2)]
    x2s = [A.alloc([128, T4], F32, "x2s") for _ in range(3)]
    b_x2s = [Buf("x2s%d" % i) for i in range(3)]
    b_out = Buf("outT")
    w_q_v = w_q.rearrange("(dc p) n -> p dc n", p=128)
    iota16 = iota_f[:, 0:16]
    c5 = {"x": 0, "wq": 0, "u": 0, "v": 0, "ab": 0, "x2": 0}

    for tt_ in range(TOWN // T4):
        tcol = slice(tt_ * T4, (tt_ + 1) * T4)

        def load_chunk(dc, p, tcol=tcol):
            k = c5["x"] % 4
            c5["x"] += 1
            S.op("sp", dma1(x1c[k][:], X1T[dc][:, tcol]), reads=[b_X1T], writes=[b_x1c[k]], dma=1)
            return x1c[k][:], b_x1c[k]
        S.op("act", lambda e: e.memzero(dummy[:]), writes=b_vS + b_G + [b_stage, b_Gall])
        norm_tile(load_chunk, T4, h2T, b_h2T, gsc2, sh2, 0, tmps4, sqs4, rst4, b_tmps4, b_sqs4, b_rst4)
        for hp in range(16):
            sl = c5["wq"] % 2
            c5["wq"] += 1
            S.op("pool", dma1(wqb[sl][:], wqT[hp].rearrange("p (dc e) -> p dc e", e=128)), reads=[b_wqT],
                 writes=[b_wqb[sl]], dma=1)
            pb = 2
            half = hp % 2

            def mmq(e, sl=sl, half=half):
                for dc in range(DC):
                    ins = e.matmul(PS[:, 2, half * 256:half * 256 + T4], lhsT=wqb[sl][:, dc, :], rhs=h2T[:, dc, :],
                                   start=(dc == 0), stop=(dc == DC - 1))
                return ins
            bq = Buf("q")
            S.op("pe", mmq, reads=[b_wqb[sl], b_stage] + b_h2T, writes=[psb[2]] if half == 0 else [],
                 pwrites=[] if half == 0 else [psb[2]])
            S.op("act", copy_fn("act", qT[:, hp, :], PS[:, 2, half * 256:half * 256 + T4]), reads=[psb[2]],
                 writes=[b_qT[hp]])
        for ts in range(NSUB):
            def mms(e, ts=ts):
                for hp in range(16):
                    ins = e.matmul(PS[:, 4 + hp // 4, (hp % 4) * 128:(hp % 4 + 1) * 128],
                                   lhsT=qT[:, hp, ts * 128:(ts + 1) * 128], rhs=skT_b[:, hp, :],
                                   start=True, stop=True)
                return ins
            S.op("pe", mms, reads=b_qT + [b_const2], writes=[psb[4], psb[5], psb[6], psb[7]])
            S.op("act", lambda e: e.copy(out=sc[:].rearrange("p a k -> p (a k)"),
                                         in_=PS[:, 4:8, :].rearrange("p b n -> p (b n)")),
                 reads=[psb[4], psb[5], psb[6], psb[7]], writes=[b_sc])
            tvv = tv[:].rearrange("p h q k -> p (h q) k")
            tiv = ti[:].rearrange("p h q k -> p (h q) k")
            NCH = 4
            for g0 in range(0, 16, NCH):
                for step in range(5):
                    for hp in range(g0, g0 + NCH):
                        kq = hp % NCH
                        if step == 0:
                            S.op("dve", lambda e, hp=hp: e.max(out=tvv[:, hp, 0:8], in_=sc[:, hp, :]),
                                 reads=[b_sc], writes=[b_tkh[hp]])
                        elif step == 1:
                            S.op("dve", lambda e, hp=hp, kq=kq: e.match_replace(
                                out=scrs[kq][:, 0:128], in_to_replace=tvv[:, hp, 0:8], in_values=sc[:, hp, :],
                                imm_value=-1e30), reads=[b_tkh[hp], b_sc], writes=[b_scrs[kq]])
                        elif step == 2:
                            S.op("dve", lambda e, hp=hp, kq=kq: e.max(out=tvv[:, hp, 8:16], in_=scrs[kq][:, 0:128]),
                                 reads=[b_scrs[kq]], pwrites=[b_tkh[hp]])
                        elif step == 3:
                            S.op("dve", lambda e, hp=hp: e.max_index(out=tiv[:, hp, 0:8], in_max=tvv[:, hp, 0:8],
                                                                      in_values=sc[:, hp, :]),
                                 reads=[b_tkh[hp], b_sc], pwrites=[b_tkh[hp]])
                        else:
                            S.op("dve", lambda e, hp=hp: e.max_index(out=tiv[:, hp, 8:16], in_max=tvv[:, hp, 8:16],
                                                                      in_values=sc[:, hp, :]),
                                 reads=[b_tkh[hp], b_sc], pwrites=[b_tkh[hp]])
            S.op("dve", lambda e: e.tensor_copy(out=tif[:], in_=ti[:]), reads=b_tkh, writes=[b_tk])
            S.op("dve", lambda e: e.tensor_tensor(
                out=cand[:], in0=tv[:, :, 0, :].unsqueeze(3).to_broadcast([128, 8, 16, 16]),
                in1=tv[:, :, 1, :].unsqueeze(2).to_broadcast([128, 8, 16, 16]), op=ALU.add),
                reads=[b_tk] + b_tkh, writes=[b_cand])
            cv = cand[:].rearrange("p h a b -> p h (a b)")
            for g0 in range(0, 8, NCH):
                for step in range(5):
                    for hh in range(g0, g0 + NCH):
                        kq = hh % NCH
                        if step == 0:
                            S.op("dve", lambda e, hh=hh: e.max(out=topv[:, hh, 0:8], in_=cv[:, hh, :]),
                                 reads=[b_cand], writes=[b_toph[hh]])
                        elif step == 1:
                            S.op("dve", lambda e, hh=hh, kq=kq: e.match_replace(
                                out=scrs[kq][:, 0:256], in_to_replace=topv[:, hh, 0:8], in_values=cv[:, hh, :],
                                imm_value=-1e30), reads=[b_toph[hh], b_cand], writes=[b_scrs[kq]])
                        elif step == 2:
                            S.op("dve", lambda e, hh=hh, kq=kq: e.max(out=topv[:, hh, 8:16], in_=scrs[kq][:, 0:256]),
                                 reads=[b_scrs[kq]], pwrites=[b_toph[hh]])
                        elif step == 3:
                            S.op("dve", lambda e, hh=hh: e.max_index(out=ci[:, hh, 0:8], in_max=topv[:, hh, 0:8],
                                                                      in_values=cv[:, hh, :]),
                                 reads=[b_toph[hh], b_cand], pwrites=[b_toph[hh]])
                        else:
                            S.op("dve", lambda e, hh=hh: e.max_index(out=ci[:, hh, 8:16], in_max=topv[:, hh, 8:16],
                                                                      in_values=cv[:, hh, :]),
                                 reads=[b_toph[hh], b_cand], pwrites=[b_toph[hh]])
            S.op("dve", lambda e: e.tensor_single_scalar(out=ca_i[:], in_=ci[:], scalar=4, op=ALU.logical_shift_right),
                 reads=[b_oh] + b_toph, writes=[b_oh])
            S.op("dve", lambda e: e.tensor_single_scalar(out=cb_i[:], in_=ci[:], scalar=15, op=ALU.bitwise_and),
                 reads=[b_oh] + b_toph, writes=[b_oh])
            S.op("dve", lambda e: e.tensor_copy(out=caf[:], in_=ca_i[:]), reads=[b_oh], writes=[b_oh])
            S.op("dve", lambda e: e.tensor_copy(out=cbf[:], in_=cb_i[:]), reads=[b_oh], writes=[b_oh])
            io4 = iota16.unsqueeze(1).unsqueeze(1).to_broadcast([128, 8, 16, 16])
            for which, sel, pp in ((0, caf, 0), (1, cbf, 1)):
                S.op("dve", lambda e, sel=sel: e.tensor_tensor(
                    out=oh[:], in0=io4, in1=sel[:].unsqueeze(3).to_broadcast([128, 8, 16, 16]), op=ALU.is_equal),
                    reads=[b_oh, b_const], writes=[b_oh])
                S.op("dve", lambda e, pp=pp: e.tensor_tensor(
                    out=oh[:], in0=oh[:], in1=tif[:, :, pp, :].unsqueeze(2).to_broadcast([128, 8, 16, 16]),
                    op=ALU.mult), reads=[b_oh, b_tk], writes=[b_oh])
                S.op("dve", lambda e, which=which: e.tensor_reduce(
                    out=egt[:, which, :].rearrange("p (h k) -> p h k", h=8), in_=oh[:], axis=AX.X, op=ALU.add),
                    reads=[b_oh], writes=[b_egt])
            g3 = egt[:, 2, :].rearrange("p (h k) -> p h k", h=8)
            S.op("dve", lambda e: e.tensor_tensor(out=g3, in0=topv[:], in1=topv[:, :, 0:1].to_broadcast([128, 8, 16]),
                                                  op=ALU.subtract), reads=[b_oh, b_egt] + b_toph, writes=[b_egt])
            S.op("act", lambda e: e.activation(out=egt[:, 2, :], in_=egt[:, 2, :], func=AF.Exp),
                 reads=[b_egt], writes=[b_egt])
            S.op("dve", lambda e: e.tensor_reduce(out=gsm[:, 0:8], in_=g3, axis=AX.X, op=ALU.add),
                 reads=[b_egt], writes=[b_egt])
            S.op("dve", lambda e: e.reciprocal(out=gsm[:, 8:16], in_=gsm[:, 0:8]), reads=[b_egt], writes=[b_egt])
            S.op("dve", lambda e: e.tensor_tensor(out=g3, in0=g3,
                                                  in1=gsm[:, 8:16].unsqueeze(2).to_broadcast([128, 8, 16]),
                                                  op=ALU.mult), reads=[b_egt], writes=[b_egt])
            def trq(e):
                for w3 in range(3):
                    ins = e.transpose(PS[:, 3, w3 * 128:(w3 + 1) * 128], egt[:, w3, :], ident_f)
                return ins
            S.op("pe", trq, reads=[b_egt, b_const], writes=[psb[3]])
            S.op("act", lambda e, ts=ts: e.copy(out=eT[:, :, ts * 128:(ts + 1) * 128],
                                               in_=PS[:, 3, 0:384].rearrange("p (w t) -> p w t", w=3)),
                 reads=[psb[3]], pwrites=[b_eT])
        iob = iota_f.unsqueeze(1).to_broadcast([128, NG, 128])
        for g0 in range(0, T4, NG):
            k = c5["ab"] % 2
            c5["ab"] += 1
            S.op("dve", lambda e, k=k, g0=g0: e.tensor_tensor(
                out=A16[k][:], in0=iob, in1=eT[:, 0, g0:g0 + NG].unsqueeze(2).to_broadcast([128, NG, 128]),
                op=ALU.is_equal), reads=[b_eT, b_const], writes=[b_AB[k]])
            S.op("dve", lambda e, k=k, g0=g0: e.tensor_tensor(
                out=A16[k][:], in0=A16[k][:], in1=eT[:, 2, g0:g0 + NG].unsqueeze(2).to_broadcast([128, NG, 128]),
                op=ALU.mult), reads=[b_eT, b_AB[k]], writes=[b_AB[k]])
            S.op("dve", lambda e, k=k, g0=g0: e.tensor_tensor(
                out=B16[k][:], in0=iob, in1=eT[:, 1, g0:g0 + NG].unsqueeze(2).to_broadcast([128, NG, 128]),
                op=ALU.is_equal), reads=[b_eT, b_const, b_AB[k]], writes=[b_AB[k]])
            for q4 in range(NG // 4):
                pb = 4 + (q4 % 2)

                def mmg(e, k=k, q4=q4, pb=pb):
                    for t4 in range(4):
                        tk_ = q4 * 4 + t4
                        ins = e.matmul(PS[:, pb, t4 * 128:(t4 + 1) * 128], lhsT=B16[k][:, tk_, :],
                                       rhs=A16[k][:, tk_, :], start=True, stop=True)
                    return ins
                S.op("pe", mmg, reads=[b_AB[k]], writes=[psb[pb]])
                t0 = g0 + q4 * 4
                S.op("act", lambda e, pb=pb, t0=t0: e.copy(
                    out=G[:, :, t0:t0 + 4].rearrange("p e t -> p t e"),
                    in_=PS[:, pb, :].rearrange("p (t e) -> p t e", t=4)),
                    reads=[psb[pb]], pwrites=[b_Gall])
        for c in range(128):
            sl = c5["u"] % nu
            c5["u"] += 1
            S.op("pool", dma1(uS[sl][:], uTb[c].rearrange("p (dc e) -> p dc e", e=128)), reads=[b_stage, b_uTb],
                 writes=[b_uS[sl]], dma=1)
            half = c % 2

            def mma(e, sl=sl, half=half):
                for dc in range(DC):
                    ins = e.matmul(PS[:, 2, half * 256:half * 256 + T4], lhsT=uS[sl][:, dc, :], rhs=h2T[:, dc, :],
                                   start=(dc == 0), stop=(dc == DC - 1))
                return ins
            S.op("pe", mma, reads=[b_uS[sl]] + b_h2T, writes=[psb[2]] if half == 0 else [],
                 pwrites=[] if half == 0 else [psb[2]])
            S.op("act", lambda e, half=half: e.activation(out=gl[half][:], in_=PS[:, 2, half * 256:half * 256 + T4],
                                                          func=AF.Gelu_apprx_tanh),
                 reads=[psb[2]], writes=[b_gl[half]])
            S.op("dve", lambda e, half=half, c=c: e.tensor_tensor(out=G[:, c, :], in0=gl[half][:], in1=G[:, c, :],
                                                                  op=ALU.mult),
                 reads=[b_gl[half], b_Gall], writes=[b_G[c]])
        S.op("act", lambda e: e.memzero(dummy[:]), writes=b_uS + b_h2T + [b_stage])
        for j in range(DC):
            half = j % 2
            for hv in range(2):
                sl = c5["v"] % 3
                c5["v"] += 1
                S.op("pool", dma1(vS[sl][:], vtb[j][:, hv * 8192:(hv + 1) * 8192].rearrange("p (c d) -> p c d", d=128)),
                     reads=[b_stage, b_vtb], writes=[b_vS[sl]], dma=1)

                def mmb(e, sl=sl, half=half, hv=hv):
                    for c in range(64):
                        ins = e.matmul(PS[:, 3, half * 256:half * 256 + T4], lhsT=vS[sl][:, c, :],
                                       rhs=G[:, hv * 64 + c, :], start=(hv == 0 and c == 0),
                                       stop=(hv == 1 and c == 63))
                    return ins
                first = (half == 0 and hv == 0)
                S.op("pe", mmb, reads=[b_vS[sl]] + b_G, writes=[psb[3]] if first else [],
                     pwrites=[] if first else [psb[3]])
            k = c5["x"] % 4
            c5["x"] += 1
            S.op("sp", dma1(x1c[k][:], X1T[j][:, tcol]), reads=[b_X1T], writes=[b_x1c[k]], dma=1)
            k2 = c5["x2"] % 3
            c5["x2"] += 1
            S.op("dve", lambda e, half=half, k=k, k2=k2, j=j: e.scalar_tensor_tensor(
                out=x2s[k2][:], in0=PS[:, 3, half * 256:half * 256 + T4], scalar=gt2[:, j:j + 1], in1=x1c[k][:],
                op0=ALU.mult, op1=ALU.add), reads=[psb[3], b_x1c[k], b_mod], writes=[b_x2s[k2]])
            S.op("act", lambda e, k2=k2: e.activation(out=sqs4[k2][:], in_=x2s[k2][:], func=AF.Square),
                 reads=[b_x2s[k2]], writes=[b_sqs4[k2]])
            S.op("pe", lambda e, k2=k2, j=j: e.matmul(PS[:, 1, 0:T4], lhsT=ones_b[:], rhs=sqs4[k2][:],
                                                      start=(j == 0), stop=(j == DC - 1)),
                 reads=[b_sqs4[k2], b_cst3], writes=[psb[1]] if j == 0 else [], pwrites=[] if j == 0 else [psb[1]])
            S.op("sp", dma1(outT[j * 128:(j + 1) * 128, tcol], x2s[k2][:]), reads=[b_x2s[k2]], pwrites=[b_out], dma=1, sb=b_x2s[k2])
        S.op("dve", lambda e: e.tensor_scalar(out=rst4[0][:], in0=PS[:, 1, 0:T4], scalar1=1.0 / D, scalar2=RMS_EPS,
                                              op0=ALU.mult, op1=ALU.add), reads=[psb[1]], writes=[b_rst4[0]])
        S.op("act", lambda e: e.activation(out=rst4[1][:], in_=rst4[0][:], func=AF.Sqrt),
             reads=[b_rst4[0]], writes=[b_rst4[1]])
        S.op("dve", lambda e: e.reciprocal(out=rst4[0][:], in_=rst4[1][:]), reads=[b_rst4[1]], writes=[b_rst4[0]])
        for j in range(DC):
            k = c5["x"] % 4
            c5["x"] += 1
            S.op("sp", dma1(x1c[k][:], outT[j * 128:(j + 1) * 128, tcol]), reads=[b_out], writes=[b_x1c[k]], dma=1)
            k2 = c5["x2"] % 3
            c5["x2"] += 1
            S.op("dve", lambda e, k=k, k2=k2, j=j: e.scalar_tensor_tensor(
                out=x2s[k2][:], in0=x1c[k][:], scalar=gfin[:, j:j + 1], in1=rst4[0][:], op0=ALU.mult, op1=ALU.mult),
                reads=[b_x1c[k], b_rst4[0], b_const], writes=[b_x2s[k2]])
            S.op("sp", dma1(outT[j * 128:(j + 1) * 128, tcol], x2s[k2][:]), reads=[b_x2s[k2]], pwrites=[b_out], dma=1, sb=b_x2s[k2])
    return finish(nc, S, outT, b_out)


def finish(nc, S, outT, b_out):
    S.barrier(final=True)
    S.emit(nc)
    return nc


def t5_bucket_np(rel):
    half = 16
    n = -rel
    ret = np.where(n < 0, half, 0)
    n = np.abs(n)
    max_exact = half // 2
    nf = np.maximum(n, 1).astype(np.float32)
    large = max_exact + (np.log(nf / np.float32(max_exact)) / np.float32(math.log(1024 / max_exact))
                         * (half - max_exact)).astype(np.int32)
    large = np.minimum(large, half - 1)
    return ret + np.where(n < max_exact, n, large)


def fm(v):
    v = np.asarray(v, np.float32)
    return np.ascontiguousarray(v.reshape(-1, 128).T)


def prep_inputs(cfg, inp, SEQ, BATCH):
    D, DC, H = cfg.D, cfg.DC, cfg.H
    x = np.asarray(inp["x"], np.float32)
    cpb = SEQ // TOWN
    ncores = BATCH * cpb
    rb = np.concatenate([np.asarray(inp["rel_bias"], np.float32), np.full((1, H), NEG, np.float32)], axis=0)
    a = np.arange(128)[:, None]
    c = np.arange(256)[None, :]
    rel = c - 64 - a
    bm = np.empty((128, H, 3, 256), np.float32)
    for br, dil in enumerate((1, 4, 16)):
        idx = np.where(np.abs(rel) <= 64, t5_bucket_np(rel * dil), 32)
        bm[:, :, br, :] = rb[idx].transpose(0, 2, 1)
    cstm = np.concatenate([np.eye(128, dtype=np.float32),
                           np.tile(np.arange(128, dtype=np.float32)[None, :], (128, 1))], axis=1)
    u = np.asarray(inp["peer_u"][0], np.float32)
    v = np.asarray(inp["peer_v"][0], np.float32)
    uTt = np.ascontiguousarray(u.reshape(128, 128, DC, 128).transpose(0, 3, 2, 1))
    vtt = np.ascontiguousarray(v.reshape(128, 128, DC, 128).transpose(2, 1, 0, 3))
    sk = np.asarray(inp["peer_subkeys"][0], np.float32)
    skT = np.ascontiguousarray(sk.reshape(16, 128, 128).transpose(2, 0, 1))
    shared = {
        "cst": cstm, "w_ada": np.ascontiguousarray(inp["w_ada"][0], np.float32),
        "w_in": np.ascontiguousarray(inp["w_in"][0], np.float32),
        "w_out": np.ascontiguousarray(inp["w_out"][0], np.float32),
        "w_q": np.ascontiguousarray(inp["peer_wq"][0], np.float32),
        "pool_w": np.ascontiguousarray(inp["pool_w"][0], np.float32),
        "skT": skT, "uTt": uTt, "vtt": vtt, "biasm": bm,
    }
    vec = np.concatenate([fm(inp["b_ada"][0]), fm(inp["g_mix"][0]), fm(inp["g_ffn"][0]), fm(inp["g_final"]),
                          fm(inp["pool_scale"][0])], axis=1)
    shared["vecs"] = np.ascontiguousarray(vec)
    maps = []
    pos = np.arange(TOWN)
    for core in range(ncores):
        b, jc = core // cpb, core % cpb
        s0 = jc * TOWN
        lo, hi = s0 - HALO, s0 + TOWN + HALO
        xs = np.zeros((TTOT, D), np.float32)
        l2, h2 = max(lo, 0), min(hi, SEQ)
        xs[l2 - lo:h2 - lo] = x[b, l2:h2]
        valid = np.zeros(TTOT, np.float32)
        valid[l2 - lo:h2 - lo] = 1.0
        vr = np.stack([valid, np.where(valid > 0, 0.0, NEG).astype(np.float32)])
        ic = np.empty((4, TOWN), np.float32)
        for g, w in enumerate((2, 4, 8, 16)):
            p = pos + s0
            ic[g] = 1.0 / (np.clip(p + w // 2, 0, SEQ) - np.clip(p - w // 2, 0, SEQ)).astype(np.float32)
        m = dict(shared)
        m["xT"] = np.ascontiguousarray(xs.T)
        m["vrows"] = vr
        m["icnt"] = ic
        m["cfm"] = fm(inp["c"][b])
        maps.append(m)
    return maps


_NC_CACHE = {}


def kernel(**inputs):
    cfg = Cfg(4096, 16)
    BATCH, SEQ = 2, 8192
    maps = prep_inputs(cfg, inputs, SEQ, BATCH)
    if "nc" not in _NC_CACHE:
        _NC_CACHE["nc"] = build_program(cfg)
    nc = _NC_CACHE["nc"]
    res = run_bass_kernel_spmd(nc, maps, core_ids=list(range(N_CORES)))
    out = np.empty((BATCH, SEQ, cfg.D), np.float32)
    cpb = SEQ // TOWN
    for core in range(N_CORES):
        b, jc = core // cpb, core % cpb
        out[b, jc * TOWN:(jc + 1) * TOWN, :] = res.results[core]["outT"].T
    return out
```

```python
import contextlib
import math

import numpy as np

import concourse.bass as bass
import concourse.mybir as mybir
from concourse.bass_utils import run_bass_kernel_spmd

F32 = mybir.dt.float32
BF16 = mybir.dt.bfloat16
U32 = mybir.dt.uint32
AF = mybir.ActivationFunctionType
ALU = mybir.AluOpType
AX = mybir.AxisListType

NEG = -30000.0
DBG_BRANCHES = (0, 1, 2)
DBG_ATT = 9
DBG_EV = 9
RMS_EPS = 1e-6
N_CORES = 8
TOWN = 2048
HALO = 1024
TTOT = 4096
PHALO = 8
PTW = TOWN + 2 * PHALO


class Buf:
    __slots__ = ("name", "w", "wprev", "pw", "pwprev", "r", "rprev", "sem", "cnt", "nobar")

    def __init__(self, name):
        self.name = name
        self.w = None
        self.wprev = None
        self.pw = []
        self.pwprev = []
        self.r = []
        self.rprev = []
        self.sem = None
        self.cnt = 0
        self.nobar = False


class Op:
    __slots__ = ("eng", "fn", "deps", "needs_inc", "token", "dma", "dbuf", "dval")

    def __init__(self, eng, fn, dma):
        self.eng = eng
        self.fn = fn
        self.dma = dma
        self.deps = set()
        self.needs_inc = False
        self.token = 0
        self.dbuf = None
        self.dval = 0


ENGS = ("pe", "act", "dve", "pool", "sp")


class Sched:
    def __init__(self):
        self.ops = {e: [] for e in ENGS}
        self.dma_bufs = []

    def op(self, eng, fn, reads=(), writes=(), pwrites=(), dma=0, sb=None):
        o = Op(eng, fn, dma)
        deps = o.deps
        for b in reads:
            if b.w is not None:
                deps.add(b.w)
            deps.update(b.pw)
        for b in writes:
            if b.w is not None:
                deps.add(b.w)
            deps.update(b.pw)
            deps.update(b.r)
        for b in pwrites:
            if b.r:
                b.rprev = b.r
                b.r = []
                b.wprev = b.w
                b.w = None
                b.pwprev = b.pw
                b.pw = []
            deps.update(b.rprev)
            deps.update(b.pwprev)
            if b.wprev is not None:
                deps.add(b.wprev)
        deps.discard(o)
        for d in deps:
            d.needs_inc = True
        for b in reads:
            b.r.append(o)
        for b in writes:
            b.w = o
            b.pw = []
            b.r = []
            b.rprev = []
            b.pwprev = []
            b.wprev = None
        for b in pwrites:
            b.pw.append(o)
        if dma:
            b = sb if sb is not None else (list(writes) + list(pwrites))[0]
            if b.sem is None:
                b.sem = len(self.dma_bufs)
                self.dma_bufs.append(b)
            b.cnt += 16 * dma
            o.dbuf = b
            o.dval = b.cnt
        self.ops[eng].append(o)
        return o

    def barrier(self, final=False):
        lasts = []
        for e in ENGS:
            for o in reversed(self.ops[e]):
                if not o.dma and o.fn is not None:
                    lasts.append(o)
                    break
        dl = {}
        for e in ENGS:
            for o in self.ops[e]:
                if o.dma and not (o.dbuf.nobar and not final):
                    dl[id(o.dbuf)] = o
        alld = lasts + list(dl.values())
        for e in ENGS:
            o = Op(e, None, 0)
            o.deps = set(alld)
            self.ops[e].append(o)
        for d in alld:
            d.needs_inc = True

    def emit(self, nc):
        for e in ENGS:
            c = 0
            for o in self.ops[e]:
                if not o.dma and o.needs_inc:
                    c += 1
                    o.token = c
        with contextlib.ExitStack() as st:
            esem = {e: st.enter_context(nc.semaphore("s_" + e)) for e in ENGS}
            dsem = [st.enter_context(nc.semaphore("d_%d" % i)) for i in range(len(self.dma_bufs))]
            block = st.enter_context(nc.Block())

            def run(e, eh):
                seen = {}
                for o in self.ops[e]:
                    waits = {}
                    for d in o.deps:
                        if d.dma:
                            key = ("d", d.dbuf.sem)
                            val = d.dval
                        else:
                            if d.eng == "pe" and e == "pe":
                                continue
                            key = ("e", d.eng)
                            val = d.token
                        if waits.get(key, 0) < val:
                            waits[key] = val
                    for key, val in waits.items():
                        if seen.get(key, 0) >= val:
                            continue
                        seen[key] = val
                        sem = dsem[key[1]] if key[0] == "d" else esem[key[1]]
                        eh.wait_ge(sem, val)
                    if o.fn is None:
                        continue
                    if o.dma:
                        o.fn(eh, dsem[o.dbuf.sem])
                    else:
                        ins = o.fn(eh)
                        if o.needs_inc:
                            ins.then_inc(esem[e], 1)

            @block.tensor
            def _(eh):
                run("pe", eh)

            @block.scalar
            def _(eh):
                run("act", eh)

            @block.vector
            def _(eh):
                run("dve", eh)

            @block.gpsimd
            def _(eh):
                run("pool", eh)

            @block.sync
            def _(eh):
                run("sp", eh)


class SBAlloc:
    def __init__(self, nc, base=0, limit=229376):
        self.nc = nc
        self.off = base
        self.limit = limit
        self.n = 0

    def alloc(self, shape, dtype, name=None):
        nbytes = int(np.prod(shape[1:])) * (2 if dtype == BF16 else 4)
        nbytes = (nbytes + 63) // 64 * 64
        self.n += 1
        nm = "%s_%d" % (name or "t", self.n)
        assert self.off + nbytes <= self.limit, (nm, self.off, nbytes)
        t = self.nc.alloc_sbuf_tensor_at(nm, list(shape), dtype, offset=self.off)
        self.off += nbytes
        return t


class Cfg:
    def __init__(self, D=4096, H=16):
        self.D = D
        self.DC = D // 128
        self.H = H
        self.AW = H * 128
        self.PW = D - self.AW
        self.PG = self.PW // 4
        self.PGC = self.PG // 128
        self.PWC = self.PW // 128
        self.MC = D // 128
        self.INW = 3 * self.AW + self.PW
        self.QD = 2048


def build_program(cfg, stop_after=None, debug=False):
    D, DC, H, AW, PW, PG, PGC, PWC, MC, INW = (cfg.D, cfg.DC, cfg.H, cfg.AW, cfg.PW, cfg.PG,
                                               cfg.PGC, cfg.PWC, cfg.MC, cfg.INW)
    nc = bass.Bass("TRN2", target_bir_lowering=False)

    def din(name, shape, dt=F32):
        return nc.dram_tensor(name, list(shape), dt, kind="ExternalInput").ap()

    def dscr(name, shape, dt):
        return nc.dram_tensor(name, list(shape), dt, kind="ExternalOutput" if debug else "Internal").ap()

    xT = din("xT", [D, TTOT])
    vrows = din("vrows", [2, TTOT])
    icnt = din("icnt", [4, TOWN])
    cfm = din("cfm", [128, DC])
    vecs = din("vecs", [128, 6 * DC + 3 * DC + PWC])
    cst = din("cst", [128, 256])
    w_ada = din("w_ada", [D, 6 * D])
    w_in = din("w_in", [D, INW])
    w_out = din("w_out", [D, D])
    w_q = din("w_q", [D, 2048])
    pool_w = din("pool_w", [4, PG, PG])
    skT = din("skT", [128, 16, 128])
    uTt = din("uTt", [128, 128, DC, 128])
    vtt = din("vtt", [DC, 128, 128, 128])
    biasm = din("biasm", [128, H, 3, 256])
    outT = nc.dram_tensor("outT", [D, TOWN], F32, kind="ExternalOutput").ap()

    QT = dscr("QT", [H, 128, TOWN], BF16)
    KT = dscr("KT", [H, 128, TTOT], BF16)
    Vs = dscr("Vs", [TTOT, AW], BF16)
    PT = dscr("PT", [PWC, 128, PTW], BF16)
    mixT = dscr("mixT", [MC, 128, TOWN], BF16)
    X1T = dscr("X1T", [DC, 128, TOWN], F32)
    uTb = nc.dram_tensor("uTb", [128, 128, DC * 128], BF16, kind="Internal").ap()
    vtb = nc.dram_tensor("vtb", [DC, 128, 128 * 128], BF16, kind="Internal").ap()
    wqT = nc.dram_tensor("wqT", [16, 128, DC * 128], BF16, kind="Internal").ap()

    dbg = {}
    if debug:
        dbg["mod"] = nc.dram_tensor("dbg_mod", [128, 6 * DC], F32, kind="ExternalOutput").ap()

    S = Sched()
    b_uTb = Buf("uTb")
    b_vtb = Buf("vtb")
    b_uTb.nobar = True
    b_vtb.nobar = True
    b_wqT = Buf("wqT")
    b_wqT.nobar = True
    PS = nc.alloc_psum_tensor("PS", [128, 8, 512], F32)
    psb = [Buf("psb%d" % i) for i in range(8)]

    CA = SBAlloc(nc, base=16512, limit=16512 + 12 * 1024)
    cst_f = CA.alloc([128, 256], F32, "cst")
    ident_f = cst_f[:, 0:128]
    iota_f = cst_f[:, 128:256]
    ident_b = CA.alloc([128, 128], BF16, "identb")
    ones_b = CA.alloc([128, 128], BF16, "onesb")
    vec_sb = CA.alloc([128, 9 * DC + PWC], F32, "vecs")
    bada = vec_sb[:, 0:6 * DC]
    gmix = vec_sb[:, 6 * DC:7 * DC]
    gffn = vec_sb[:, 7 * DC:8 * DC]
    gfin = vec_sb[:, 8 * DC:9 * DC]
    pscale = vec_sb[:, 9 * DC:9 * DC + PWC]
    mod = CA.alloc([128, 6 * DC], F32, "mod")
    gsc1 = CA.alloc([128, DC], F32, "gsc1")
    gsc2 = CA.alloc([128, DC], F32, "gsc2")
    c_sb = CA.alloc([128, DC], F32, "c")
    cs_b = CA.alloc([128, DC], BF16, "cs")
    skT_b = CA.alloc([128, 16, 128], BF16, "skT")
    sh1, sc1, gt1 = mod[:, 0:DC], mod[:, DC:2 * DC], mod[:, 2 * DC:3 * DC]
    sh2, sc2, gt2 = mod[:, 3 * DC:4 * DC], mod[:, 4 * DC:5 * DC], mod[:, 5 * DC:6 * DC]
    CBASE = 16512 + 12 * 1024
    b_const = Buf("const")
    b_const2 = Buf("const2")
    b_mod = Buf("mod")

    def dma1(out, in_, **kw):
        def fn(eh, sem):
            eh.dma_start(out=out, in_=in_, **kw).then_inc(sem, 16)
        return fn

    def dman(pairs, **kw):
        def fn(eh, sem):
            for (o, i) in pairs:
                eh.dma_start(out=o, in_=i, **kw).then_inc(sem, 16)
        return fn

    S.op("sp", dman([(cst_f[:], cst[:, :]), (vec_sb[:], vecs[:, :]), (c_sb[:], cfm[:, :])]),
         writes=[b_const], dma=3)
    S.op("pool", dma1(skT_b[:], skT[:, :, :]), writes=[b_const2], dma=1)
    S.op("dve", lambda e: e.tensor_copy(out=ident_b[:], in_=ident_f), reads=[b_const], writes=[Buf("identb")])
    b_cst3 = Buf("const3")
    S.op("dve", lambda e: e.memset(ones_b[:], 1.0), writes=[b_cst3])
    b_cs = Buf("cs")
    S.op("act", lambda e: e.activation(out=cs_b[:], in_=c_sb[:], func=AF.Silu), reads=[b_const], writes=[b_cs])

    rr = [0]

    def evac_eng():
        rr[0] += 1
        return "act" if rr[0] % 2 else "dve"

    def copy_fn(eng, out, in_):
        if eng == "act":
            return lambda e: e.copy(out=out, in_=in_)
        return lambda e: e.tensor_copy(out=out, in_=in_)

    A = SBAlloc(nc, base=CBASE)
    NB = 512
    nblk = 6 * D // NB
    wa = [A.alloc([128, DC, NB], BF16, "wa") for _ in range(2)]
    b_wa = [Buf("wa%d" % i) for i in range(2)]
    modrow = A.alloc([1, 6 * D], F32, "modrow")
    b_modrow = Buf("modrow")
    w_ada_v = w_ada.rearrange("(dc p) n -> p dc n", p=128)
    hdc = max(1, DC // 2)
    for nb in range(nblk):
        sl = nb % 2
        pairs = []
        for q in range(0, DC, hdc):
            pairs.append((wa[sl][:, q:q + hdc, :], w_ada_v[:, q:q + hdc, nb * NB:(nb + 1) * NB]))
        S.op("pool", dman(pairs), writes=[b_wa[sl]], dma=len(pairs))
        pb = 1 + nb % 2

        def mmA(e, sl=sl, pb=pb):
            for dc in range(DC):
                ins = e.matmul(PS[0:1, pb, :], lhsT=cs_b[:, dc:dc + 1], rhs=wa[sl][:, dc, :],
                               start=(dc == 0), stop=(dc == DC - 1))
            return ins
        S.op("pe", mmA, reads=[b_wa[sl], b_cs], writes=[psb[pb]])
        eng = evac_eng()
        S.op(eng, copy_fn(eng, modrow[0:1, nb * NB:(nb + 1) * NB], PS[0:1, pb, :]),
             reads=[psb[pb]], pwrites=[b_modrow])

    def mmT(e):
        for jj in range(6 * DC):
            ins = e.matmul(PS[:, 3, jj:jj + 1], lhsT=modrow[0:1, jj * 128:(jj + 1) * 128],
                           rhs=ident_f[0:1, 0:1], start=True, stop=True)
        return ins
    S.op("pe", mmT, reads=[b_modrow, b_const], writes=[psb[3]])
    S.op("dve", lambda e: e.tensor_tensor(out=mod[:], in0=PS[:, 3, 0:6 * DC], in1=bada, op=ALU.add),
         reads=[psb[3], b_const], writes=[b_mod])
    S.op("dve", lambda e: e.scalar_tensor_tensor(out=gsc1[:], in0=sc1, scalar=1.0, in1=gmix,
                                                 op0=ALU.add, op1=ALU.mult),
         reads=[b_mod], writes=[Buf("gsc1")])
    b_gsc = Buf("gsc")
    S.op("dve", lambda e: e.scalar_tensor_tensor(out=gsc2[:], in0=sc2, scalar=1.0, in1=gffn,
                                                 op0=ALU.add, op1=ALU.mult),
         reads=[b_mod], writes=[b_gsc])
    if debug:
        b_dbg = Buf("dbg")
        S.op("sp", dma1(dbg["mod"][:, :], mod[:]), reads=[b_mod, b_gsc], writes=[b_dbg], dma=1)
    S.barrier()
    if stop_after == "A":
        return finish(nc, S, outT, None)

    def norm_tile(load_chunk, NT, hT, b_hT_chunks, gsc, sh, ss_bank, tmps, sqs, rst, b_tmps, b_sqs, b_rst,
                  xbufs=None):
        nsq = len(sqs)
        for dc in range(DC):
            xa, xb = load_chunk(dc, 0)
            k = dc % nsq
            S.op("act", lambda e, xa=xa, k=k: e.activation(out=sqs[k][:, 0:NT], in_=xa, func=AF.Square),
                 reads=[xb], writes=[b_sqs[k]])
            S.op("pe", lambda e, k=k, dc=dc: e.matmul(PS[:, ss_bank, 0:NT], lhsT=ones_b[:], rhs=sqs[k][:, 0:NT],
                                                      start=(dc == 0), stop=(dc == DC - 1)),
                 reads=[b_sqs[k], b_cst3], writes=[psb[ss_bank]] if dc == 0 else [], pwrites=[] if dc == 0 else [psb[ss_bank]])
        S.op("dve", lambda e: e.tensor_scalar(out=rst[0][:, 0:NT], in0=PS[:, ss_bank, 0:NT], scalar1=1.0 / D,
                                              scalar2=RMS_EPS, op0=ALU.mult, op1=ALU.add),
             reads=[psb[ss_bank]], writes=[b_rst[0]])
        S.op("act", lambda e: e.activation(out=rst[1][:, 0:NT], in_=rst[0][:, 0:NT], func=AF.Sqrt),
             reads=[b_rst[0]], writes=[b_rst[1]])
        S.op("dve", lambda e: e.reciprocal(out=rst[0][:, 0:NT], in_=rst[1][:, 0:NT]),
             reads=[b_rst[1]], writes=[b_rst[0]])
        nt = len(tmps)
        for dc in range(DC):
            xa, xb = load_chunk(dc, 1)
            k = dc % nt
            S.op("dve", lambda e, xa=xa, k=k, dc=dc: e.scalar_tensor_tensor(
                out=tmps[k][:, 0:NT], in0=xa, scalar=gsc[:, dc:dc + 1], in1=rst[0][:, 0:NT],
                op0=ALU.mult, op1=ALU.mult),
                reads=[xb, b_rst[0], b_gsc], writes=[b_tmps[k]])
            S.op("act", lambda e, k=k, dc=dc: e.activation(out=hT[:, dc, 0:NT], in_=tmps[k][:, 0:NT],
                                                           func=AF.Identity, bias=sh[:, dc:dc + 1]),
                 reads=[b_tmps[k], b_mod], writes=[b_hT_chunks[dc]])

    A = SBAlloc(nc, base=CBASE)
    NT1 = 512
    WB = 256
    xt = A.alloc([128, DC, NT1], F32, "xt")
    b_xt = Buf("xt")
    hTs = [A.alloc([128, DC, NT1], BF16, "hT") for _ in range(2)]
    b_hT = [[Buf("hT%d_%d" % (s, dc)) for dc in range(DC)] for s in range(2)]
    wbs = [A.alloc([128, DC, WB], BF16, "wb") for _ in range(3)]
    b_wb = [Buf("wb%d" % i) for i in range(3)]
    sqs = [A.alloc([128, NT1], BF16, "sq") for _ in range(4)]
    b_sqs = [Buf("sq%d" % i) for i in range(4)]
    tmps = [A.alloc([128, NT1], F32, "tmp") for _ in range(3)]
    b_tmps = [Buf("tmp%d" % i) for i in range(3)]
    rst = [A.alloc([128, NT1], F32, "rst") for _ in range(2)]
    b_rst = [Buf("rst%d" % i) for i in range(2)]
    stg = [A.alloc([128, NT1], BF16, "stg") for _ in range(4)]
    b_stg = [Buf("stg%d" % i) for i in range(4)]
    b_QT, b_KT, b_Vs, b_PT = Buf("QT"), Buf("KT"), Buf("Vs"), Buf("PT")
    xT_v = xT.rearrange("(dc p) t -> p dc t", p=128)
    w_in_v = w_in.rearrange("(dc p) n -> p dc n", p=128)
    ntiles = TTOT // NT1
    cnt = {"w": 0, "ps": 0, "stg": 0}

    def load_x(i):
        pairs = []
        q4 = max(1, DC // 4)
        for q in range(0, DC, q4):
            pairs.append((xt[:, q:q + q4, :], xT_v[:, q:q + q4, i * NT1:(i + 1) * NT1]))
        S.op("sp", dman(pairs), writes=[b_xt], dma=len(pairs))

    def norm1(i):
        s = i % 2
        norm_tile(lambda dc, p: (xt[:, dc, :], b_xt), NT1, hTs[s], b_hT[s], gsc1, sh1, 0,
                  tmps, sqs, rst, b_tmps, b_sqs, b_rst)

    def proj1(i):
        s = i % 2
        own = 2 <= i <= 5
        hT = hTs[s]
        for cb in range(INW // WB):
            c0 = cb * WB
            kind = "q" if c0 < AW else "k" if c0 < 2 * AW else "v" if c0 < 3 * AW else "p"
            tok = None
            if kind in ("q",) and not own:
                continue
            if kind == "p" and not own:
                if i == 1:
                    tok = (NT1 - PHALO, NT1, 0)
                elif i == 6:
                    tok = (0, PHALO, PHALO + TOWN)
                else:
                    continue
            sl = cnt["w"] % 3
            cnt["w"] += 1
            pairs = []
            for q in range(0, DC, hdc):
                pairs.append((wbs[sl][:, q:q + hdc, :], w_in_v[:, q:q + hdc, c0:c0 + WB]))
            S.op("pool", dman(pairs), writes=[b_wb[sl]], dma=len(pairs))
            if kind == "v":
                for ts in range(NT1 // 128):
                    pb = 1 + cnt["ps"] % 4
                    cnt["ps"] += 1

                    def mmv(e, sl=sl, pb=pb, ts=ts, hT=hT):
                        for dc in range(DC):
                            ins = e.matmul(PS[:, pb, 0:WB], lhsT=hT[:, dc, ts * 128:(ts + 1) * 128],
                                           rhs=wbs[sl][:, dc, :], start=(dc == 0), stop=(dc == DC - 1))
                        return ins
                    S.op("pe", mmv, reads=[b_wb[sl]] + b_hT[s], writes=[psb[pb]])
                    k = cnt["stg"] % 4
                    cnt["stg"] += 1
                    eng = evac_eng()
                    S.op(eng, copy_fn(eng, stg[k][:, 0:WB], PS[:, pb, 0:WB]), reads=[psb[pb]], writes=[b_stg[k]])
                    r0 = i * NT1 + ts * 128
                    S.op("sp", dma1(Vs[r0:r0 + 128, c0 - 2 * AW:c0 - 2 * AW + WB], stg[k][:, 0:WB]),
                         reads=[b_stg[k]], pwrites=[b_Vs], dma=1, sb=b_stg[k])
            else:
                for nt in range(WB // 128):
                    pb = 1 + cnt["ps"] % 4
                    cnt["ps"] += 1
                    t0, t1 = (0, NT1) if tok is None else (tok[0], tok[1])
                    n = t1 - t0

                    def mmf(e, sl=sl, pb=pb, nt=nt, hT=hT, t0=t0, t1=t1, n=n):
                        for dc in range(DC):
                            ins = e.matmul(PS[:, pb, 0:n], lhsT=wbs[sl][:, dc, nt * 128:(nt + 1) * 128],
                                           rhs=hT[:, dc, t0:t1], start=(dc == 0), stop=(dc == DC - 1))
                        return ins
                    S.op("pe", mmf, reads=[b_wb[sl]] + b_hT[s], writes=[psb[pb]])
                    k = cnt["stg"] % 4
                    cnt["stg"] += 1
                    eng = evac_eng()
                    S.op(eng, copy_fn(eng, stg[k][:, 0:n], PS[:, pb, 0:n]), reads=[psb[pb]], writes=[b_stg[k]])
                    col = c0 + nt * 128
                    if kind == "q":
                        hh = col // 128
                        dst, bb = QT[hh][:, (i - 2) * NT1:(i - 1) * NT1], b_QT
                    elif kind == "k":
                        hh = (col - AW) // 128
                        dst, bb = KT[hh][:, i * NT1:(i + 1) * NT1], b_KT
                    else:
                        ch = (col - 3 * AW) // 128
                        if tok is None:
                            dst = PT[ch][:, PHALO + (i - 2) * NT1:PHALO + (i - 1) * NT1]
                        else:
                            dst = PT[ch][:, tok[2]:tok[2] + PHALO]
                        bb = b_PT
                    S.op("sp", dma1(dst, stg[k][:, 0:n]), reads=[b_stg[k]], pwrites=[bb], dma=1, sb=b_stg[k])

    load_x(0)
    norm1(0)
    for i in range(ntiles):
        if i + 1 < ntiles:
            load_x(i + 1)
            norm1(i + 1)
        proj1(i)
    S.barrier()
    if stop_after == "1":
        return finish(nc, S, outT, None)

    A = SBAlloc(nc, base=CBASE)
    scale = 128.0 ** -0.5
    KTh = [A.alloc([128, TTOT], BF16, "KTh") for _ in range(2)]
    QTh = [A.alloc([128, TOWN], BF16, "QTh") for _ in range(2)]
    V1 = [A.alloc([128, 17, 128], BF16, "V1") for _ in range(2)]
    V4 = [A.alloc([128, 4, 5, 128], BF16, "V4") for _ in range(2)]
    V16 = [A.alloc([128, 16, 2, 128], BF16, "V16") for _ in range(2)]
    bia = [A.alloc([128, 3, 256], F32, "bias") for _ in range(2)]
    b_hd = [Buf("hd%d" % i) for i in range(2)]
    num = [A.alloc([128, TOWN], F32, "num") for _ in range(2)]
    den = [A.alloc([128, TOWN], F32, "den") for _ in range(2)]
    b_num = [Buf("num%d" % i) for i in range(2)]
    b_den = [Buf("den%d" % i) for i in range(2)]
    vneg = A.alloc([128, TTOT], F32, "vneg")
    b_vneg = Buf("vneg")
    NSL = 4
    Ls = [A.alloc([128, 256], F32, "L") for _ in range(NSL)]
    b_L = [Buf("L%d" % i) for i in range(NSL)]
    Pb = [A.alloc([128, 256], BF16, "P") for _ in range(NSL)]
    b_P = [Buf("P%d" % i) for i in range(NSL)]
    PTs = [A.alloc([128, 256], BF16, "PTs") for _ in range(NSL)]
    b_PTs = [Buf("PTs%d" % i) for i in range(NSL)]
    ast = [A.alloc([128, TOWN], BF16, "ast") for _ in range(2)]
    b_ast = [Buf("ast%d" % i) for i in range(2)]
    b_mixT = Buf("mixT")
    PSb = PS[:, :, :].bitcast(BF16)

    S.op("sp", dma1(vneg[:], vrows[1:2, :].partition_broadcast(128).rearrange("p o t -> p (o t)")), writes=[b_vneg], dma=1)
    uT_src = uTt.rearrange("c p dc e -> (c p) (dc e)").rearrange("r (a k) -> (r a) k", k=min(2048, DC * 128))
    uT_dst = uTb.rearrange("c p n -> (c p) n").rearrange("r (a k) -> (r a) k", k=min(2048, DC * 128))
    v_src = vtt.rearrange("j p c d -> (j p) (c d)").rearrange("r (a k) -> (r a) k", k=2048)
    v_dst = vtb.rearrange("j p n -> (j p) n").rearrange("r (a k) -> (r a) k", k=2048)
    pre_pieces = []
    stepu = 1024
    for r0 in range(0, uT_src.shape[0], stepu):
        pre_pieces.append((uT_dst[r0:r0 + stepu, :], uT_src[r0:r0 + stepu, :], b_uTb))
    for r0 in range(0, v_src.shape[0], stepu):
        pre_pieces.append((v_dst[r0:r0 + stepu, :], v_src[r0:r0 + stepu, :], b_vtb))
    wq_src = w_q.rearrange("(dc p) (hp e) -> hp p dc e", p=128, e=128)
    for hp in range(16):
        pre_pieces.append((wqT[hp].rearrange("p (dc e) -> p dc e", e=128), wq_src[hp], b_wqT))
    pre_state = {"i": 0}

    def emit_pre(upto):
        while pre_state["i"] < min(upto, len(pre_pieces)):
            d_, s_, b_ = pre_pieces[pre_state["i"]]
            pre_state["i"] += 1
            S.op("pool", dma1(d_, s_), pwrites=[b_], dma=1, sb=b_)

    def load_head(h):
        s = h % 2
        hc = slice(h * 128, (h + 1) * 128)
        pairs = [
            (KTh[s][:], KT[h]),
            (QTh[s][:], QT[h]),
            (V1[s][:], Vs[960:960 + 17 * 128, hc].rearrange("(kc p) d -> p kc d", p=128)),
            (bia[s][:], biasm[:, h]),
        ]
        for r in range(4):
            pairs.append((V4[s][:, r], Vs[768:768 + 5 * 512, hc].rearrange("(kc p r) d -> p r kc d", p=128, r=4)[:, r]))
        for r in range(16):
            pairs.append((V16[s][:, r], Vs[0:4096, hc].rearrange("(kc p r) d -> p r kc d", p=128, r=16)[:, r]))
        S.op("sp", dman(pairs), reads=[b_QT, b_KT, b_Vs], writes=[b_hd[s]], dma=len(pairs))

    def finish_head(h):
        s = h % 2
        S.op("dve", lambda e, s=s: e.reciprocal(out=den[s][:], in_=den[s][:]), reads=[b_den[s]], writes=[b_den[s]])
        S.op("dve", lambda e, s=s: e.tensor_tensor(out=ast[s][:], in0=num[s][:], in1=den[s][:], op=ALU.mult),
             reads=[b_den[s], b_num[s]], writes=[b_ast[s]])
        S.op("sp", dma1(mixT[h], ast[s][:]), reads=[b_ast[s]], pwrites=[b_mixT], dma=1, sb=b_ast[s])
        if h + 2 < H:
            load_head(h + 2)

    tiles = []
    for h in range(H):
        first = True
        for br, dil in enumerate((1, 4, 16)):
            for r in range(dil):
                for m in range(16 // dil):
                    tiles.append(dict(h=h, br=br, dil=dil, r=r, m=m, q=len(tiles), last=False))
        tiles[-1]["last"] = True

    def st0(t):
        h, br, dil, r, m, q = t["h"], t["br"], t["dil"], t["r"], t["m"], t["q"]
        s = h % 2
        k4 = q % NSL
        sb_ = q % 2
        qs = r + dil * 128 * m
        ks = r + HALO + dil * (128 * m - 64)
        q_ap = QTh[s][:, qs:qs + dil * 127 + 1:dil]
        k_ap = KTh[s][:, ks:ks + dil * 255 + 1:dil]
        vn_ap = vneg[:, ks:ks + dil * 255 + 1:dil]
        S_ps = PS[:, sb_, 0:256]
        S.op("pe", lambda e: e.matmul(S_ps, lhsT=q_ap, rhs=k_ap, start=True, stop=True),
             reads=[b_hd[s]], writes=[psb[sb_]])
        S.op("dve", lambda e: e.scalar_tensor_tensor(out=Ls[k4][:], in0=S_ps, scalar=scale, in1=bia[s][:, br, :],
                                                     op0=ALU.mult, op1=ALU.add),
             reads=[psb[sb_], b_hd[s]], writes=[b_L[k4]])
        S.op("pool", lambda e: e.tensor_tensor(out=Ls[k4][:], in0=Ls[k4][:], in1=vn_ap, op=ALU.add),
             reads=[b_vneg, b_L[k4]], writes=[b_L[k4]])
        S.op("act", lambda e: e.activation(out=Pb[k4][:], in_=Ls[k4][:], func=AF.Exp),
             reads=[b_L[k4]], writes=[b_P[k4]])

    def st1(t):
        q = t["q"]
        k4 = q % NSL
        tb = 2 + q % 2
        PT_ps = PSb[:, tb, 0:256]

        def trf(e):
            e.transpose(PT_ps[:, 0:128], Pb[k4][:, 0:128], ident_b[:])
            return e.transpose(PT_ps[:, 128:256], Pb[k4][:, 128:256], ident_b[:])
        S.op("pe", trf, reads=[b_P[k4]], writes=[psb[tb]])
        S.op("act", lambda e: e.copy(out=PTs[k4][:], in_=PT_ps), reads=[psb[tb]], writes=[b_PTs[k4]])

    def st2(t):
        h, br, dil, r, m, q = t["h"], t["br"], t["dil"], t["r"], t["m"], t["q"]
        s = h % 2
        k4 = q % NSL
        ob = 4 + q % 2
        qs = r + dil * 128 * m
        if dil == 1:
            v0, v1 = V1[s][:, m, :], V1[s][:, m + 1, :]
        elif dil == 4:
            v0, v1 = V4[s][:, r, m, :], V4[s][:, r, m + 1, :]
        else:
            v0, v1 = V16[s][:, r, 0, :], V16[s][:, r, 1, :]

        def pvf(e):
            e.matmul(PS[:, ob, 0:128], lhsT=v0, rhs=PTs[k4][:, 0:128], start=True, stop=False)
            e.matmul(PS[:, ob, 0:128], lhsT=v1, rhs=PTs[k4][:, 128:256], start=False, stop=True)
            e.matmul(PS[:, ob + 2, 0:128], lhsT=ones_b[:], rhs=PTs[k4][:, 0:128], start=True, stop=False)
            return e.matmul(PS[:, ob + 2, 0:128], lhsT=ones_b[:], rhs=PTs[k4][:, 128:256], start=False, stop=True)
        S.op("pe", pvf, reads=[b_PTs[k4], b_hd[s], b_cst3], writes=[psb[ob], psb[ob + 2]])
        n_ap = num[s][:, qs:qs + dil * 127 + 1:dil]
        d_ap = den[s][:, qs:qs + dil * 127 + 1:dil]
        if br == 0:
            S.op("dve", lambda e: e.tensor_copy(out=n_ap, in_=PS[:, ob, 0:128]), reads=[psb[ob]], pwrites=[b_num[s]])
            S.op("dve", lambda e: e.tensor_copy(out=d_ap, in_=PS[:, ob + 2, 0:128]), reads=[psb[ob + 2]],
                 pwrites=[b_den[s]])
        else:
            S.op("dve", lambda e: e.tensor_tensor(out=n_ap, in0=PS[:, ob, 0:128], in1=n_ap, op=ALU.add),
                 reads=[psb[ob], b_num[s]], pwrites=[b_num[s]])
            S.op("dve", lambda e: e.tensor_tensor(out=d_ap, in0=PS[:, ob + 2, 0:128], in1=d_ap, op=ALU.add),
                 reads=[psb[ob + 2], b_den[s]], pwrites=[b_den[s]])
        if t["last"]:
            finish_head(h)

    load_head(0)
    if H > 1:
        load_head(1)
    nt_ = len(tiles)
    for i in range(nt_ + 2):
        emit_pre((i + 1) * len(pre_pieces) // max(1, nt_ - 8) + 1)
        if i < nt_:
            st0(tiles[i])
        if 0 <= i - 1 < nt_:
            st1(tiles[i - 1])
        if 0 <= i - 2 < nt_:
            st2(tiles[i - 2])
    S.barrier()
    if stop_after == "2":
        return finish(nc, S, outT, None)

    A = SBAlloc(nc, base=CBASE)
    v01 = A.alloc([128, PTW], F32, "v01")
    b_v01 = Buf("v01")
    icn = [A.alloc([128, TOWN], F32, "icn") for _ in range(2)]
    b_icn = [Buf("icn%d" % i) for i in range(2)]
    ptl = [A.alloc([128, PTW], BF16, "ptl") for _ in range(2)]
    b_ptl = [Buf("ptl%d" % i) for i in range(2)]
    pm = [A.alloc([128, PTW], F32, "pm") for _ in range(2)]
    sA = [A.alloc([128, PTW], F32, "sA") for _ in range(2)]
    sB = [A.alloc([128, PTW], F32, "sB") for _ in range(2)]
    b_pl = [Buf("pl%d" % i) for i in range(2)]
    pooled = [A.alloc([128, PGC, TOWN], BF16, "pooled") for _ in range(2)]
    b_pooled = [[Buf("pooled%d_%d" % (i, c)) for c in range(PGC)] for i in range(2)]
    wp = [A.alloc([128, PGC, PG], BF16, "wp") for _ in range(2)]
    b_wp = [Buf("wp%d" % i) for i in range(2)]
    stg3 = [A.alloc([128, 512], BF16, "stg3") for _ in range(3)]
    b_stg3 = [Buf("stg3_%d" % i) for i in range(3)]
    S.op("sp", dma1(v01[:], vrows[0:1, HALO - PHALO:HALO + TOWN + PHALO].partition_broadcast(128).rearrange("p o t -> p (o t)")),
         writes=[b_v01], dma=1)
    c3 = {"ps": 0, "stg": 0, "ch": 0}
    for g in range(4):
        gs = g % 2
        S.op("sp", dma1(icn[gs][:], icnt[g:g + 1, :].partition_broadcast(128).rearrange("p o t -> p (o t)")), writes=[b_icn[gs]], dma=1)
        S.op("pool", dma1(wp[gs][:], pool_w[g].rearrange("(kc p) n -> p kc n", p=128)), writes=[b_wp[gs]], dma=1)
        for pc in range(PGC):
            ch = g * PGC + pc
            k = c3["ch"] % 2
            c3["ch"] += 1
            eng = "dve" if k == 0 else "pool"
            S.op("sp", dma1(ptl[k][:], PT[ch]), reads=[b_PT], writes=[b_ptl[k]], dma=1)
            E = PTW

            def tt(out, a, b, op):
                return lambda e: e.tensor_tensor(out=out, in0=a, in1=b, op=op)
            S.op(eng, tt(pm[k][:], ptl[k][:], v01[:], ALU.mult), reads=[b_ptl[k], b_v01], writes=[b_pl[k]])
            S.op(eng, tt(sA[k][:, 1:E], pm[k][:, 1:E], pm[k][:, 0:E - 1], ALU.add), reads=[b_pl[k]], writes=[b_pl[k]])
            cur, oth = sA[k], sB[k]
            lo, hi, sh_ = 1, E, 1
            for lvl in range(g):
                nlo, nhi = lo + sh_, hi - sh_
                S.op(eng, tt(oth[:, nlo:nhi], cur[:, nlo + sh_:nhi + sh_], cur[:, nlo - sh_:nhi - sh_], ALU.add),
                     reads=[b_pl[k]], writes=[b_pl[k]])
                cur, oth = oth, cur
                lo, hi = nlo, nhi
                sh_ *= 2
            assert lo <= PHALO and hi >= PHALO + TOWN, (lo, hi)
            S.op(eng, tt(oth[:, 0:TOWN], cur[:, PHALO:PHALO + TOWN], icn[gs][:], ALU.mult),
                 reads=[b_pl[k], b_icn[gs]], writes=[b_pl[k]])
            S.op(eng, tt(pooled[gs][:, pc, :], oth[:, 0:TOWN], pm[k][:, PHALO:PHALO + TOWN], ALU.subtract),
                 reads=[b_pl[k]], writes=[b_pooled[gs][pc]])
        for jo in range(PGC):
            for tb in range(TOWN // 512):
                pb = 1 + c3["ps"] % 4
                c3["ps"] += 1

                def mmp(e, gs=gs, jo=jo, tb=tb, pb=pb):
                    for kc in range(PGC):
                        ins = e.matmul(PS[:, pb, :], lhsT=wp[gs][:, kc, jo * 128:(jo + 1) * 128],
                                       rhs=pooled[gs][:, kc, tb * 512:(tb + 1) * 512],
                                       start=(kc == 0), stop=(kc == PGC - 1))
                    return ins
                S.op("pe", mmp, reads=[b_wp[gs]] + b_pooled[gs], writes=[psb[pb]])
                k = c3["stg"] % 3
                c3["stg"] += 1
                col = g * PGC + jo
                S.op("act", lambda e, k=k, pb=pb, col=col: e.activation(out=stg3[k][:], in_=PS[:, pb, :],
                                                                        func=AF.Identity,
                                                                        scale=pscale[:, col:col + 1]),
                     reads=[psb[pb], b_const], writes=[b_stg3[k]])
                S.op("sp", dma1(mixT[H + col][:, tb * 512:(tb + 1) * 512], stg3[k][:]),
                     reads=[b_stg3[k]], pwrites=[b_mixT], dma=1, sb=b_stg3[k])
    S.barrier()

    A = SBAlloc(nc, base=CBASE)
    TB2 = 1024
    mxl = A.alloc([128, MC, TB2], BF16, "mxl")
    b_mxl = Buf("mxl")
    wo = [A.alloc([128, MC, WB], BF16, "wo") for _ in range(3)]
    b_wo = [Buf("wo%d" % i) for i in range(3)]
    xo = [A.alloc([128, 512], F32, "xo") for _ in range(4)]
    b_xo = [Buf("xo%d" % i) for i in range(4)]
    x1s = [A.alloc([128, 512], F32, "x1s") for _ in range(4)]
    b_x1s = [Buf("x1s%d" % i) for i in range(4)]
    b_X1T = Buf("X1T")
    mixT_v = mixT.rearrange("c p t -> p c t")
    w_out_v = w_out.rearrange("(mc p) n -> p mc n", p=128)
    c4 = {"w": 0, "ps": 0, "x": 0}
    for tb in range(TOWN // TB2):
        pairs = []
        q4 = max(1, MC // 4)
        for q in range(0, MC, q4):
            pairs.append((mxl[:, q:q + q4, :], mixT_v[:, q:q + q4, tb * TB2:(tb + 1) * TB2]))
        S.op("sp", dman(pairs), reads=[b_mixT], writes=[b_mxl], dma=len(pairs))
        for cb in range(D // WB):
            sl = c4["w"] % 3
            c4["w"] += 1
            pairs = []
            for q in range(0, MC, hdc):
                pairs.append((wo[sl][:, q:q + hdc, :], w_out_v[:, q:q + hdc, cb * WB:(cb + 1) * WB]))
            S.op("pool", dman(pairs), writes=[b_wo[sl]], dma=len(pairs))
            for nt in range(WB // 128):
                j = cb * (WB // 128) + nt
                for hf in range(TB2 // 512):
                    t0 = tb * TB2 + hf * 512
                    pb = 1 + c4["ps"] % 4
                    c4["ps"] += 1
                    k = c4["x"] % 4
                    c4["x"] += 1
                    S.op("sp", dma1(xo[k][:], xT[j * 128:(j + 1) * 128, HALO + t0:HALO + t0 + 512]),
                         writes=[b_xo[k]], dma=1)

                    def mmo(e, sl=sl, nt=nt, hf=hf, pb=pb):
                        for mc in range(MC):
                            ins = e.matmul(PS[:, pb, :], lhsT=wo[sl][:, mc, nt * 128:(nt + 1) * 128],
                                           rhs=mxl[:, mc, hf * 512:(hf + 1) * 512], start=(mc == 0), stop=(mc == MC - 1))
                        return ins
                    S.op("pe", mmo, reads=[b_wo[sl], b_mxl], writes=[psb[pb]])
                    S.op("dve", lambda e, k=k, pb=pb, j=j: e.scalar_tensor_tensor(
                        out=x1s[k][:], in0=PS[:, pb, :], scalar=gt1[:, j:j + 1], in1=xo[k][:],
                        op0=ALU.mult, op1=ALU.add),
                        reads=[psb[pb], b_xo[k], b_mod], writes=[b_x1s[k]])
                    S.op("sp", dma1(X1T[j][:, t0:t0 + 512], x1s[k][:]), reads=[b_x1s[k]], pwrites=[b_X1T], dma=1,
                         sb=b_x1s[k])
    S.barrier()
    if stop_after == "3":
        return finish(nc, S, outT, None)

    A = SBAlloc(nc, base=CBASE)
    T4 = 256
    NSUB = T4 // 128
    G = A.alloc([128, 128, T4], BF16, "G")
    b_G = [Buf("G%d" % c) for c in range(128)]
    b_Gall = Buf("Gall")
    stage_base = A.off
    vS = [A.alloc([128, 64, 128], BF16, "vS") for _ in range(3)]
    b_vS = [Buf("vS%d" % i) for i in range(3)]
    after_stage = A.off
    A.off = stage_base
    h2T = A.alloc([128, DC, T4], BF16, "h2T")
    b_h2T = [Buf("h2T_%d" % dc) for dc in range(DC)]
    nu = 4
    uS = [A.alloc([128, DC, 128], BF16, "uS") for _ in range(nu)]
    b_uS = [Buf("uS%d" % i) for i in range(nu)]
    wq_s = uS
    assert A.off <= after_stage, (A.off, after_stage)
    b_stage = Buf("stage")
    A.off = after_stage
    wqb = [A.alloc([128, DC, 128], BF16, "wqb") for _ in range(2)]
    b_wqb = [Buf("wqb%d" % i) for i in range(2)]
    qT = A.alloc([128, 16, T4], BF16, "qT")
    b_qT = [Buf("qT%d" % i) for i in range(16)]
    sc = A.alloc([128, 16, 128], F32, "sc")
    b_sc = Buf("sc")
    scrs = [A.alloc([128, 256], F32, "scr") for _ in range(4)]
    b_scrs = [Buf("scr%d" % i) for i in range(4)]
    b_tkh = [Buf("tkh%d" % i) for i in range(16)]
    b_toph = [Buf("toph%d" % i) for i in range(8)]
    tv = A.alloc([128, 8, 2, 16], F32, "tv")
    ti = A.alloc([128, 8, 2, 16], U32, "ti")
    tif = A.alloc([128, 8, 2, 16], F32, "tif")
    b_tk = Buf("tk")
    cand = A.alloc([128, 8, 16, 16], F32, "cand")
    b_cand = Buf("cand")
    topv = A.alloc([128, 8, 16], F32, "topv")
    ci = A.alloc([128, 8, 16], U32, "ci")
    ca_i = A.alloc([128, 8, 16], U32, "ca_i")
    cb_i = A.alloc([128, 8, 16], U32, "cb_i")
    caf = A.alloc([128, 8, 16], F32, "caf")
    cbf = A.alloc([128, 8, 16], F32, "cbf")
    oh = sc[:].rearrange("p a k -> p (a k)").rearrange("p (h a b) -> p h a b", h=8, a=16)
    b_oh = b_sc
    egt = A.alloc([128, 3, 128], F32, "egt")
    b_egt = Buf("egt")
    gsm = A.alloc([128, 16], F32, "gsm")
    dummy = A.alloc([128, 2], F32, "dummy")
    eT = A.alloc([128, 3, T4], F32, "eT")
    b_eT = Buf("eT")
    NG = 8
    A16 = [A.alloc([128, NG, 128], BF16, "A16") for _ in range(2)]
    B16 = [A.alloc([128, NG, 128], BF16, "B16") for _ in range(2)]
    b_AB = [Buf("AB%d" % i) for i in range(2)]
    x1c = [A.alloc([128, T4], F32, "x1c") for _ in range(4)]
    b_x1c = [Buf("x1c%d" % i) for i in range(4)]
    sqs4 = [A.alloc([128, T4], BF16, "sq4") for _ in range(3)]
    b_sqs4 = [Buf("sq4_%d" % i) for i in range(3)]
    tmps4 = [A.alloc([128, T4], F32, "tmp4") for _ in range(3)]
    b_tmps4 = [Buf("tmp4_%d" % i) for i in range(3)]
    rst4 = [A.alloc([128, T4], F32, "rst4") for _ in range(2)]
    b_rst4 = [Buf("rst4_%d" % i) for i in range(2)]
    gl = [A.alloc([128, T4], F32, "gl") for _ in range(2)]
    b_gl = [Buf("gl%d" % i) for i in range(2)]
    x2s = [A.alloc([128, T4], F32, "x2s") for _ in range(3)]
    b_x2s = [Buf("x2s%d" % i) for i in range(3)]
    b_out = Buf("outT")
    w_q_v = w_q.rearrange("(dc p) n -> p dc n", p=128)
    iota16 = iota_f[:, 0:16]
    c5 = {"x": 0, "wq": 0, "u": 0, "v": 0, "ab": 0, "x2": 0}

    for tt_ in range(TOWN // T4):
        tcol = slice(tt_ * T4, (tt_ + 1) * T4)

        def load_chunk(dc, p, tcol=tcol):
            k = c5["x"] % 4
            c5["x"] += 1
            S.op("sp", dma1(x1c[k][:], X1T[dc][:, tcol]), reads=[b_X1T], writes=[b_x1c[k]], dma=1)
            return x1c[k][:], b_x1c[k]
        S.op("act", lambda e: e.memzero(dummy[:]), writes=b_vS + b_G + [b_stage, b_Gall])
        norm_tile(load_chunk, T4, h2T, b_h2T, gsc2, sh2, 0, tmps4, sqs4, rst4, b_tmps4, b_sqs4, b_rst4)
        for hp in range(16):
            sl = c5["wq"] % 2
            c5["wq"] += 1
            S.op("pool", dma1(wqb[sl][:], wqT[hp].rearrange("p (dc e) -> p dc e", e=128)), reads=[b_wqT],
                 writes=[b_wqb[sl]], dma=1)
            pb = 2
            half = hp % 2

            def mmq(e, sl=sl, half=half):
                for dc in range(DC):
                    ins = e.matmul(PS[:, 2, half * 256:half * 256 + T4], lhsT=wqb[sl][:, dc, :], rhs=h2T[:, dc, :],
                                   start=(dc == 0), stop=(dc == DC - 1))
                return ins
            bq = Buf("q")
            S.op("pe", mmq, reads=[b_wqb[sl], b_stage] + b_h2T, writes=[psb[2]] if half == 0 else [],
                 pwrites=[] if half == 0 else [psb[2]])
            S.op("act", copy_fn("act", qT[:, hp, :], PS[:, 2, half * 256:half * 256 + T4]), reads=[psb[2]],
                 writes=[b_qT[hp]])
        for ts in range(NSUB):
            def mms(e, ts=ts):
                for hp in range(16):
                    ins = e.matmul(PS[:, 4 + hp // 4, (hp % 4) * 128:(hp % 4 + 1) * 128],
                                   lhsT=qT[:, hp, ts * 128:(ts + 1) * 128], rhs=skT_b[:, hp, :],
                                   start=True, stop=True)
                return ins
            S.op("pe", mms, reads=b_qT + [b_const2], writes=[psb[4], psb[5], psb[6], psb[7]])
            S.op("act", lambda e: e.copy(out=sc[:].rearrange("p a k -> p (a k)"),
                                         in_=PS[:, 4:8, :].rearrange("p b n -> p (b n)")),
                 reads=[psb[4], psb[5], psb[6], psb[7]], writes=[b_sc])
            tvv = tv[:].rearrange("p h q k -> p (h q) k")
            tiv = ti[:].rearrange("p h q k -> p (h q) k")
            NCH = 4
            for g0 in range(0, 16, NCH):
                for step in range(5):
                    for hp in range(g0, g0 + NCH):
                        kq = hp % NCH
                        if step == 0:
                            S.op("dve", lambda e, hp=hp: e.max(out=tvv[:, hp, 0:8], in_=sc[:, hp, :]),
                                 reads=[b_sc], writes=[b_tkh[hp]])
                        elif step == 1:
                            S.op("dve", lambda e, hp=hp, kq=kq: e.match_replace(
                                out=scrs[kq][:, 0:128], in_to_replace=tvv[:, hp, 0:8], in_values=sc[:, hp, :],
                                imm_value=-1e30), reads=[b_tkh[hp], b_sc], writes=[b_scrs[kq]])
                        elif step == 2:
                            S.op("dve", lambda e, hp=hp, kq=kq: e.max(out=tvv[:, hp, 8:16], in_=scrs[kq][:, 0:128]),
                                 reads=[b_scrs[kq]], pwrites=[b_tkh[hp]])
                        elif step == 3:
                            S.op("dve", lambda e, hp=hp: e.max_index(out=tiv[:, hp, 0:8], in_max=tvv[:, hp, 0:8],
                                                                      in_values=sc[:, hp, :]),
                                 reads=[b_tkh[hp], b_sc], pwrites=[b_tkh[hp]])
                        else:
                            S.op("dve", lambda e, hp=hp: e.max_index(out=tiv[:, hp, 8:16], in_max=tvv[:, hp, 8:16],
                                                                      in_values=sc[:, hp, :]),
                                 reads=[b_tkh[hp], b_sc], pwrites=[b_tkh[hp]])
            S.op("dve", lambda e: e.tensor_copy(out=tif[:], in_=ti[:]), reads=b_tkh, writes=[b_tk])
            S.op("dve", lambda e: e.tensor_tensor(
                out=cand[:], in0=tv[:, :, 0, :].unsqueeze(3).to_broadcast([128, 8, 16, 16]),
                in1=tv[:, :, 1, :].unsqueeze(2).to_broadcast([128, 8, 16, 16]), op=ALU.add),
                reads=[b_tk] + b_tkh, writes=[b_cand])
            cv = cand[:].rearrange("p h a b -> p h (a b)")
            for g0 in range(0, 8, NCH):
                for step in range(5):
                    for hh in range(g0, g0 + NCH):
                        kq = hh % NCH
                        if step == 0:
                            S.op("dve", lambda e, hh=hh: e.max(out=topv[:, hh, 0:8], in_=cv[:, hh, :]),
                                 reads=[b_cand], writes=[b_toph[hh]])
                        elif step == 1:
                            S.op("dve", lambda e, hh=hh, kq=kq: e.match_replace(
                                out=scrs[kq][:, 0:256], in_to_replace=topv[:, hh, 0:8], in_values=cv[:, hh, :],
                                imm_value=-1e30), reads=[b_toph[hh], b_cand], writes=[b_scrs[kq]])
                        elif step == 2:
                            S.op("dve", lambda e, hh=hh, kq=kq: e.max(out=topv[:, hh, 8:16], in_=scrs[kq][:, 0:256]),
                                 reads=[b_scrs[kq]], pwrites=[b_toph[hh]])
                        elif step == 3:
                            S.op("dve", lambda e, hh=hh: e.max_index(out=ci[:, hh, 0:8], in_max=topv[:, hh, 0:8],
                                                                      in_values=cv[:, hh, :]),
                                 reads=[b_toph[hh], b_cand], pwrites=[b_toph[hh]])
                        else:
                            S.op("dve", lambda e, hh=hh: e.max_index(out=ci[:, hh, 8:16], in_max=topv[:, hh, 8:16],
                                                                      in_values=cv[:, hh, :]),
                                 reads=[b_toph[hh], b_cand], pwrites=[b_toph[hh]])
            S.op("dve", lambda e: e.tensor_single_scalar(out=ca_i[:], in_=ci[:], scalar=4, op=ALU.logical_shift_right),
                 reads=[b_oh] + b_toph, writes=[b_oh])
            S.op("dve", lambda e: e.tensor_single_scalar(out=cb_i[:], in_=ci[:], scalar=15, op=ALU.bitwise_and),
                 reads=[b_oh] + b_toph, writes=[b_oh])
            S.op("dve", lambda e: e.tensor_copy(out=caf[:], in_=ca_i[:]), reads=[b_oh], writes=[b_oh])
            S.op("dve", lambda e: e.tensor_copy(out=cbf[:], in_=cb_i[:]), reads=[b_oh], writes=[b_oh])
            io4 = iota16.unsqueeze(1).unsqueeze(1).to_broadcast([128, 8, 16, 16])
            for which, sel, pp in ((0, caf, 0), (1, cbf, 1)):
                S.op("dve", lambda e, sel=sel: e.tensor_tensor(
                    out=oh[:], in0=io4, in1=sel[:].unsqueeze(3).to_broadcast([128, 8, 16, 16]), op=ALU.is_equal),
                    reads=[b_oh, b_const], writes=[b_oh])
                S.op("dve", lambda e, pp=pp: e.tensor_tensor(
                    out=oh[:], in0=oh[:], in1=tif[:, :, pp, :].unsqueeze(2).to_broadcast([128, 8, 16, 16]),
                    op=ALU.mult), reads=[b_oh, b_tk], writes=[b_oh])
                S.op("dve", lambda e, which=which: e.tensor_reduce(
                    out=egt[:, which, :].rearrange("p (h k) -> p h k", h=8), in_=oh[:], axis=AX.X, op=ALU.add),
                    reads=[b_oh], writes=[b_egt])
            g3 = egt[:, 2, :].rearrange("p (h k) -> p h k", h=8)
            S.op("dve", lambda e: e.tensor_tensor(out=g3, in0=topv[:], in1=topv[:, :, 0:1].to_broadcast([128, 8, 16]),
                                                  op=ALU.subtract), reads=[b_oh, b_egt] + b_toph, writes=[b_egt])
            S.op("act", lambda e: e.activation(out=egt[:, 2, :], in_=egt[:, 2, :], func=AF.Exp),
                 reads=[b_egt], writes=[b_egt])
            S.op("dve", lambda e: e.tensor_reduce(out=gsm[:, 0:8], in_=g3, axis=AX.X, op=ALU.add),
                 reads=[b_egt], writes=[b_egt])
            S.op("dve", lambda e: e.reciprocal(out=gsm[:, 8:16], in_=gsm[:, 0:8]), reads=[b_egt], writes=[b_egt])
            S.op("dve", lambda e: e.tensor_tensor(out=g3, in0=g3,
                                                  in1=gsm[:, 8:16].unsqueeze(2).to_broadcast([128, 8, 16]),
                                                  op=ALU.mult), reads=[b_egt], writes=[b_egt])
            def trq(e):
                for w3 in range(3):
                    ins = e.transpose(PS[:, 3, w3 * 128:(w3 + 1) * 128], egt[:, w3, :], ident_f)
                return ins
            S.op("pe", trq, reads=[b_egt, b_const], writes=[psb[3]])
            S.op("act", lambda e, ts=ts: e.copy(out=eT[:, :, ts * 128:(ts + 1) * 128],
                                               in_=PS[:, 3, 0:384].rearrange("p (w t) -> p w t", w=3)),
                 reads=[psb[3]], pwrites=[b_eT])
        iob = iota_f.unsqueeze(1).to_broadcast([128, NG, 128])
        for g0 in range(0, T4, NG):
            k = c5["ab"] % 2
            c5["ab"] += 1
            S.op("dve", lambda e, k=k, g0=g0: e.tensor_tensor(
                out=A16[k][:], in0=iob, in1=eT[:, 0, g0:g0 + NG].unsqueeze(2).to_broadcast([128, NG, 128]),
                op=ALU.is_equal), reads=[b_eT, b_const], writes=[b_AB[k]])
            S.op("dve", lambda e, k=k, g0=g0: e.tensor_tensor(
                out=A16[k][:], in0=A16[k][:], in1=eT[:, 2, g0:g0 + NG].unsqueeze(2).to_broadcast([128, NG, 128]),
                op=ALU.mult), reads=[b_eT, b_AB[k]], writes=[b_AB[k]])
            S.op("dve", lambda e, k=k, g0=g0: e.tensor_tensor(
                out=B16[k][:], in0=iob, in1=eT[:, 1, g0:g0 + NG].unsqueeze(2).to_broadcast([128, NG, 128]),
                op=ALU.is_equal), reads=[b_eT, b_const, b_AB[k]], writes=[b_AB[k]])
            for q4 in range(NG // 4):
                pb = 4 + (q4 % 2)

                def mmg(e, k=k, q4=q4, pb=pb):
                    for t4 in range(4):
                        tk_ = q4 * 4 + t4
                        ins = e.matmul(PS[:, pb, t4 * 128:(t4 + 1) * 128], lhsT=B16[k][:, tk_, :],
                                       rhs=A16[k][:, tk_, :], start=True, stop=True)
                    return ins
                S.op("pe", mmg, reads=[b_AB[k]], writes=[psb[pb]])
                t0 = g0 + q4 * 4
                S.op("act", lambda e, pb=pb, t0=t0: e.copy(
                    out=G[:, :, t0:t0 + 4].rearrange("p e t -> p t e"),
                    in_=PS[:, pb, :].rearrange("p (t e) -> p t e", t=4)),
                    reads=[psb[pb]], pwrites=[b_Gall])
        for c in range(128):
            sl = c5["u"] % nu
            c5["u"] += 1
            S.op("pool", dma1(uS[sl][:], uTb[c].rearrange("p (dc e) -> p dc e", e=128)), reads=[b_stage, b_uTb],
                 writes=[b_uS[sl]], dma=1)
            half = c % 2

            def mma(e, sl=sl, half=half):
                for dc in range(DC):
                    ins = e.matmul(PS[:, 2, half * 256:half * 256 + T4], lhsT=uS[sl][:, dc, :], rhs=h2T[:, dc, :],
                                   start=(dc == 0), stop=(dc == DC - 1))
                return ins
            S.op("pe", mma, reads=[b_uS[sl]] + b_h2T, writes=[psb[2]] if half == 0 else [],
                 pwrites=[] if half == 0 else [psb[2]])
            S.op("act", lambda e, half=half: e.activation(out=gl[half][:], in_=PS[:, 2, half * 256:half * 256 + T4],
                                                          func=AF.Gelu_apprx_tanh),
                 reads=[psb[2]], writes=[b_gl[half]])
            S.op("dve", lambda e, half=half, c=c: e.tensor_tensor(out=G[:, c, :], in0=gl[half][:], in1=G[:, c, :],
                                                                  op=ALU.mult),
                 reads=[b_gl[half], b_Gall], writes=[b_G[c]])
        S.op("act", lambda e: e.memzero(dummy[:]), writes=b_uS + b_h2T + [b_stage])
        for j in range(DC):
            half = j % 2
            for hv in range(2):
                sl = c5["v"] % 3
                c5["v"] += 1
                S.op("pool", dma1(vS[sl][:], vtb[j][:, hv * 8192:(hv + 1) * 8192].rearrange("p (c d) -> p c d", d=128)),
                     reads=[b_stage, b_vtb], writes=[b_vS[sl]], dma=1)

                def mmb(e, sl=sl, half=half, hv=hv):
                    for c in range(64):
                        ins = e.matmul(PS[:, 3, half * 256:half * 256 + T4], lhsT=vS[sl][:, c, :],
                                       rhs=G[:, hv * 64 + c, :], start=(hv == 0 and c == 0),
                                       stop=(hv == 1 and c == 63))
                    return ins
                first = (half == 0 and hv == 0)
                S.op("pe", mmb, reads=[b_vS[sl]] + b_G, writes=[psb[3]] if first else [],
                     pwrites=[] if first else [psb[3]])
            k = c5["x"] % 4
            c5["x"] += 1
            S.op("sp", dma1(x1c[k][:], X1T[j][:, tcol]), reads=[b_X1T], writes=[b_x1c[k]], dma=1)
            k2 = c5["x2"] % 3
            c5["x2"] += 1
            S.op("dve", lambda e, half=half, k=k, k2=k2, j=j: e.scalar_tensor_tensor(
                out=x2s[k2][:], in0=PS[:, 3, half * 256:half * 256 + T4], scalar=gt2[:, j:j + 1], in1=x1c[k][:],
                op0=ALU.mult, op1=ALU.add), reads=[psb[3], b_x1c[k], b_mod], writes=[b_x2s[k2]])
            S.op("act", lambda e, k2=k2: e.activation(out=sqs4[k2][:], in_=x2s[k2][:], func=AF.Square),
                 reads=[b_x2s[k2]], writes=[b_sqs4[k2]])
            S.op("pe", lambda e, k2=k2, j=j: e.matmul(PS[:, 1, 0:T4], lhsT=ones_b[:], rhs=sqs4[k2][:],
                                                      start=(j == 0), stop=(j == DC - 1)),
                 reads=[b_sqs4[k2], b_cst3], writes=[psb[1]] if j == 0 else [], pwrites=[] if j == 0 else [psb[1]])
            S.op("sp", dma1(outT[j * 128:(j + 1) * 128, tcol], x2s[k2][:]), reads=[b_x2s[k2]], pwrites=[b_out], dma=1, sb=b_x2s[k2])
        S.op("dve", lambda e: e.tensor_scalar(out=rst4[0][:], in0=PS[:, 1, 0:T4], scalar1=1.0 / D, scalar2=RMS_EPS,
                                              op0=ALU.mult, op1=ALU.add), reads=[psb[1]], writes=[b_rst4[0]])
        S.op("act", lambda e: e.activation(out=rst4[1][:], in_=rst4[0][:], func=AF.Sqrt),
             reads=[b_rst4[0]], writes=[b_rst4[1]])
        S.op("dve", lambda e: e.reciprocal(out=rst4[0][:], in_=rst4[1][:]), reads=[b_rst4[1]], writes=[b_rst4[0]])
        for j in range(DC):
            k = c5["x"] % 4
            c5["x"] += 1
            S.op("sp", dma1(x1c[k][:], outT[j * 128:(j + 1) * 128, tcol]), reads=[b_out], writes=[b_x1c[k]], dma=1)
            k2 = c5["x2"] % 3
            c5["x2"] += 1
            S.op("dve", lambda e, k=k, k2=k2, j=j: e.scalar_tensor_tensor(
                out=x2s[k2][:], in0=x1c[k][:], scalar=gfin[:, j:j + 1], in1=rst4[0][:], op0=ALU.mult, op1=ALU.mult),
                reads=[b_x1c[k], b_rst4[0], b_const], writes=[b_x2s[k2]])
            S.op("sp", dma1(outT[j * 128:(j + 1) * 128, tcol], x2s[k2][:]), reads=[b_x2s[k2]], pwrites=[b_out], dma=1, sb=b_x2s[k2])
    return finish(nc, S, outT, b_out)


def finish(nc, S, outT, b_out):
    S.barrier(final=True)
    S.emit(nc)
    return nc


def t5_bucket_np(rel):
    half = 16
    n = -rel
    ret = np.where(n < 0, half, 0)
    n = np.abs(n)
    max_exact = half // 2
    nf = np.maximum(n, 1).astype(np.float32)
    large = max_exact + (np.log(nf / np.float32(max_exact)) / np.float32(math.log(1024 / max_exact))
                         * (half - max_exact)).astype(np.int32)
    large = np.minimum(large, half - 1)
    return ret + np.where(n < max_exact, n, large)


def fm(v):
    v = np.asarray(v, np.float32)
    return np.ascontiguousarray(v.reshape(-1, 128).T)


def prep_inputs(cfg, inp, SEQ, BATCH):
    D, DC, H = cfg.D, cfg.DC, cfg.H
    x = np.asarray(inp["x"], np.float32)
    cpb = SEQ // TOWN
    ncores = BATCH * cpb
    rb = np.concatenate([np.asarray(inp["rel_bias"], np.float32), np.full((1, H), NEG, np.float32)], axis=0)
    a = np.arange(128)[:, None]
    c = np.arange(256)[None, :]
    rel = c - 64 - a
    bm = np.empty((128, H, 3, 256), np.float32)
    for br, dil in enumerate((1, 4, 16)):
        idx = np.where(np.abs(rel) <= 64, t5_bucket_np(rel * dil), 32)
        bm[:, :, br, :] = rb[idx].transpose(0, 2, 1)
    cstm = np.concatenate([np.eye(128, dtype=np.float32),
                           np.tile(np.arange(128, dtype=np.float32)[None, :], (128, 1))], axis=1)
    u = np.asarray(inp["peer_u"][0], np.float32)
    v = np.asarray(inp["peer_v"][0], np.float32)
    uTt = np.ascontiguousarray(u.reshape(128, 128, DC, 128).transpose(0, 3, 2, 1))
    vtt = np.ascontiguousarray(v.reshape(128, 128, DC, 128).transpose(2, 1, 0, 3))
    sk = np.asarray(inp["peer_subkeys"][0], np.float32)
    skT = np.ascontiguousarray(sk.reshape(16, 128, 128).transpose(2, 0, 1))
    shared = {
        "cst": cstm, "w_ada": np.ascontiguousarray(inp["w_ada"][0], np.float32),
        "w_in": np.ascontiguousarray(inp["w_in"][0], np.float32),
        "w_out": np.ascontiguousarray(inp["w_out"][0], np.float32),
        "w_q": np.ascontiguousarray(inp["peer_wq"][0], np.float32),
        "pool_w": np.ascontiguousarray(inp["pool_w"][0], np.float32),
        "skT": skT, "uTt": uTt, "vtt": vtt, "biasm": bm,
    }
    vec = np.concatenate([fm(inp["b_ada"][0]), fm(inp["g_mix"][0]), fm(inp["g_ffn"][0]), fm(inp["g_final"]),
                          fm(inp["pool_scale"][0])], axis=1)
    shared["vecs"] = np.ascontiguousarray(vec)
    maps = []
    pos = np.arange(TOWN)
    for core in range(ncores):
        b, jc = core // cpb, core % cpb
        s0 = jc * TOWN
        lo, hi = s0 - HALO, s0 + TOWN + HALO
        xs = np.zeros((TTOT, D), np.float32)
        l2, h2 = max(lo, 0), min(hi, SEQ)
        xs[l2 - lo:h2 - lo] = x[b, l2:h2]
        valid = np.zeros(TTOT, np.float32)
        valid[l2 - lo:h2 - lo] = 1.0
        vr = np.stack([valid, np.where(valid > 0, 0.0, NEG).astype(np.float32)])
        ic = np.empty((4, TOWN), np.float32)
        for g, w in enumerate((2, 4, 8, 16)):
            p = pos + s0
            ic[g] = 1.0 / (np.clip(p + w // 2, 0, SEQ) - np.clip(p - w // 2, 0, SEQ)).astype(np.float32)
        m = dict(shared)
        m["xT"] = np.ascontiguousarray(xs.T)
        m["vrows"] = vr
        m["icnt"] = ic
        m["cfm"] = fm(inp["c"][b])
        maps.append(m)
    return maps


_NC_CACHE = {}


def kernel(**inputs):
    cfg = Cfg(4096, 16)
    BATCH, SEQ = 2, 8192
    maps = prep_inputs(cfg, inputs, SEQ, BATCH)
    if "nc" not in _NC_CACHE:
        _NC_CACHE["nc"] = build_program(cfg)
    nc = _NC_CACHE["nc"]
    res = run_bass_kernel_spmd(nc, maps, core_ids=list(range(N_CORES)))
    out = np.empty((BATCH, SEQ, cfg.D), np.float32)
    cpb = SEQ // TOWN
    for core in range(N_CORES):
        b, jc = core // cpb, core % cpb
        out[b, jc * TOWN:(jc + 1) * TOWN, :] = res.results[core]["outT"].T
    return out
```
